# Optimizing a Trainium2 kernel written in Bass

```python
import math
import jax, jax.numpy as jnp
from jax import lax
import numpy as np

D_MODEL = 4096
BATCH = 1
SEQ = 8192
DEPTH = 2

HEAD_DIM = 128
MIX_WIDTH = D_MODEL
SB_HEADS = 16
SB_WIDTH = SB_HEADS * HEAD_DIM
SSM_WIDTH = MIX_WIDTH - SB_WIDTH
SSM_GROUP = 16
SSM_GROUPS = SSM_WIDTH // SSM_GROUP
SSM_STATE = 64
SSM_DT_MIN = 1e-3
SSM_DT_MAX = 1e-1
HGRN_HEADS = 16
HGRN_WIDTH = HGRN_HEADS * HEAD_DIM
HGRN_CHUNK = 64
MOBA_HEADS = 16
MOBA_WIDTH = MOBA_HEADS * HEAD_DIM
MOBA_BLOCK = 256
MOBA_TOPK = 3
ROPE_THETA = 500000.0
ROPE_DIM = HEAD_DIM // 4
Q_BLOCK = 128
D_FF = 11008
ADA_ROWS = 9
EVEN_IN = 3 * SB_WIDTH + SSM_WIDTH
ODD_IN = 4 * HGRN_WIDTH + 3 * MOBA_WIDTH
N_EVEN = (DEPTH + 1) // 2
N_ODD = DEPTH // 2
RMS_EPS = 1e-6
NEG = -1e30

kernel_name = "hybrid_sb_s5_hgrn2_moba_macaron"


def rms_norm(x, gain):
    xf = x.astype(jnp.float32)
    y = xf * lax.rsqrt(jnp.mean(xf * xf, axis=-1, keepdims=True) + RMS_EPS)
    return (y * gain.astype(jnp.float32)).astype(x.dtype)


def swiglu(h, w1, w3, w2):
    return (jax.nn.silu(h @ w1) * (h @ w3)) @ w2


def sandwich(x, fn, g_pre, g_post, shift, scale, gate, res_w):
    h = rms_norm(x, g_pre) * (1.0 + scale[:, None, :]) + shift[:, None, :]
    y = rms_norm(fn(h), g_post)
    return x + res_w * gate[:, None, :] * y


def partial_rope(t, pos):
    half = ROPE_DIM // 2
    inv = ROPE_THETA ** (-jnp.arange(half, dtype=jnp.float32) / half)
    ang = pos.astype(jnp.float32)[:, None] * inv[None, :]
    cos = jnp.cos(ang)[None, :, None, :]
    sin = jnp.sin(ang)[None, :, None, :]
    tr = t[..., :ROPE_DIM].astype(jnp.float32)
    t1, t2 = tr[..., :half], tr[..., half:]
    rot = jnp.concatenate([t1 * cos - t2 * sin, t2 * cos + t1 * sin], axis=-1)
    return jnp.concatenate([rot.astype(t.dtype), t[..., ROPE_DIM:]], axis=-1)


def stick_breaking_attention(q, k, v):
    B, H, S, Dh = q.shape
    scale = HEAD_DIM ** -0.5
    kpos = jnp.arange(S)

    def block(i):
        qb = lax.dynamic_slice_in_dim(q, i * Q_BLOCK, Q_BLOCK, axis=2)
        qpos = i * Q_BLOCK + jnp.arange(Q_BLOCK)
        z = jnp.einsum('bhtd,bhsd->bhts', qb, k).astype(jnp.float32) * scale
        mask = kpos[None, :] < qpos[:, None]
        log_keep = jnp.where(mask, jax.nn.log_sigmoid(-z), 0.0)
        tail = lax.cumsum(log_keep, axis=3, reverse=True)
        excl = jnp.concatenate([tail[..., 1:], jnp.zeros_like(tail[..., :1])], axis=3)
        w = jnp.where(mask, jnp.exp(jax.nn.log_sigmoid(z) + excl), 0.0)
        return jnp.einsum('bhts,bhsd->bhtd', w.astype(v.dtype), v)

    out = lax.map(block, jnp.arange(S // Q_BLOCK))
    return jnp.moveaxis(out, 0, 2).reshape(B, H, S, Dh)


def s5_mixer(u, a_re, a_im, b_re, b_im, c_re, c_im, d_skip, log_dt, glu_w):
    Bsz, S, _ = u.shape
    f32 = jnp.float32
    ug = u.reshape(Bsz, S, SSM_GROUPS, SSM_GROUP).astype(f32)
    dt = jnp.exp(log_dt.astype(f32))[:, None]
    ar, ai = a_re.astype(f32), a_im.astype(f32)
    mag = jnp.exp(ar * dt)
    lr, li = mag * jnp.cos(ai * dt), mag * jnp.sin(ai * dt)
    den = ar * ar + ai * ai
    nr, ni = lr - 1.0, li
    cr = (nr * ar + ni * ai) / den
    ci = (ni * ar - nr * ai) / den
    br, bi = b_re.astype(f32), b_im.astype(f32)
    bbr = cr[..., None] * br - ci[..., None] * bi
    bbi = cr[..., None] * bi + ci[..., None] * br
    xr = jnp.einsum('bsgh,gph->bsgp', ug, bbr)
    xi = jnp.einsum('bsgh,gph->bsgp', ug, bbi)
    lam_r = jnp.broadcast_to(lr, xr.shape)
    lam_i = jnp.broadcast_to(li, xr.shape)

    def combine(e1, e2):
        a1r, a1i, b1r, b1i = e1
        a2r, a2i, b2r, b2i = e2
        return (a2r * a1r - a2i * a1i, a2r * a1i + a2i * a1r,
                a2r * b1r - a2i * b1i + b2r, a2r * b1i + a2i * b1r + b2i)

    _, _, hr, hi = lax.associative_scan(combine, (lam_r, lam_i, xr, xi), axis=1)
    y = (jnp.einsum('gnp,bsgp->bsgn', c_re.astype(f32), hr)
         - jnp.einsum('gnp,bsgp->bsgn', c_im.astype(f32), hi))
    y = y + d_skip.astype(f32).reshape(SSM_GROUPS, SSM_GROUP) * ug
    y = jax.nn.gelu(y.reshape(Bsz, S, SSM_WIDTH))
    y = y * jax.nn.sigmoid(y @ glu_w.astype(f32))
    return y.astype(u.dtype)


def hgrn2_mixer(q, f_logit, i_in, gate, lb, norm_w):
    Bsz, S, _ = q.shape
    f32 = jnp.float32
    n_chunks = S // HGRN_CHUNK
    f = lb + (1.0 - lb) * jax.nn.sigmoid(f_logit.astype(f32))
    log_f = jnp.log(f)
    kk = 1.0 - f

    def to_chunks(t):
        t = t.astype(f32).reshape(Bsz, n_chunks, HGRN_CHUNK, HGRN_HEADS, HEAD_DIM)
        return t.transpose(1, 0, 3, 2, 4)

    qc, kc, gc, vc = to_chunks(q), to_chunks(kk), to_chunks(log_f), to_chunks(i_in)
    causal = jnp.tril(jnp.ones((HGRN_CHUNK, HGRN_CHUNK), dtype=bool))

    def step(state, inp):
        qb, kb, gb, vb = inp
        G = jnp.cumsum(gb, axis=2)
        o_inter = jnp.einsum('bhtk,bhkv->bhtv', qb * jnp.exp(G), state)
        diff = G[:, :, :, None, :] - G[:, :, None, :, :]
        decay = jnp.exp(jnp.where(causal[:, :, None], diff, -jnp.inf))
        att = jnp.einsum('bhtk,bhsk,bhtsk->bhts', qb, kb, decay)
        o = o_inter + jnp.einsum('bhts,bhsv->bhtv', att, vb)
        k_dec = kb * jnp.exp(G[:, :, -1:, :] - G)
        new_state = (state * jnp.exp(G[:, :, -1, :])[..., None]
                     + jnp.einsum('bhsk,bhsv->bhkv', k_dec, vb))
        return new_state, o

    init = jnp.zeros((Bsz, HGRN_HEADS, HEAD_DIM, HEAD_DIM), f32)
    _, o = lax.scan(step, init, (qc, kc, gc, vc))
    o = o.transpose(1, 0, 3, 2, 4).reshape(Bsz, S, HGRN_HEADS, HEAD_DIM)
    o = o * lax.rsqrt(jnp.mean(o * o, axis=-1, keepdims=True) + RMS_EPS) * norm_w.astype(f32)
    g = jax.nn.silu(gate.astype(f32)).reshape(Bsz, S, HGRN_HEADS, HEAD_DIM)
    return (o * g).reshape(Bsz, S, HGRN_WIDTH).astype(gate.dtype)


def moba_attention(q, k, v):
    B, H, S, Dh = q.shape
    nkb = -(-S // MOBA_BLOCK)
    pad = nkb * MOBA_BLOCK - S
    kp = jnp.pad(k, ((0, 0), (0, 0), (0, pad), (0, 0)))
    vp = jnp.pad(v, ((0, 0), (0, 0), (0, pad), (0, 0)))
    kblk = kp.reshape(B, H, nkb, MOBA_BLOCK, Dh)
    vblk = vp.reshape(B, H, nkb, MOBA_BLOCK, Dh)
    kmean = jnp.mean(kblk.astype(jnp.float32), axis=3)
    n_sel = max(1, min(MOBA_TOPK, nkb - 1))
    scale = HEAD_DIM ** -0.5
    b_idx = jnp.arange(B)[:, None, None]
    h_idx = jnp.arange(H)[None, :, None]

    def block(i):
        q0 = i * Q_BLOCK
        qb = lax.dynamic_slice_in_dim(q, q0, Q_BLOCK, axis=2)
        qpos = q0 + jnp.arange(Q_BLOCK)
        own = q0 // MOBA_BLOCK
        gate = jnp.einsum('bhtd,bhnd->bhtn', qb.astype(jnp.float32), kmean)
        past = jnp.arange(nkb) < own
        gate = jnp.where(past, gate, NEG)
        _, idx = lax.top_k(gate, n_sel)
        slot_ok = jnp.arange(n_sel) < own
        scores, vsel = [], []
        for j in range(n_sel):
            kg = kblk[b_idx, h_idx, idx[..., j]]
            sj = jnp.einsum('bhtd,bhtkd->bhtk', qb, kg).astype(jnp.float32) * scale
            scores.append(jnp.where(slot_ok[j], sj, NEG))
            vsel.append(vblk[b_idx, h_idx, idx[..., j]])
        ko = lax.dynamic_slice_in_dim(kp, own * MOBA_BLOCK, MOBA_BLOCK, axis=2)
        vo = lax.dynamic_slice_in_dim(vp, own * MOBA_BLOCK, MOBA_BLOCK, axis=2)
        kpos = own * MOBA_BLOCK + jnp.arange(MOBA_BLOCK)
        so = jnp.einsum('bhtd,bhkd->bhtk', qb, ko).astype(jnp.float32) * scale
        so = jnp.where(kpos[None, :] <= qpos[:, None], so, NEG)
        p = jax.nn.softmax(jnp.concatenate(scores + [so], axis=-1), axis=-1)
        p = p.reshape(B, H, Q_BLOCK, n_sel + 1, MOBA_BLOCK).astype(v.dtype)
        out = jnp.einsum('bhtk,bhkd->bhtd', p[:, :, :, n_sel, :], vo)
        for j in range(n_sel):
            out = out + jnp.einsum('bhtk,bhtkd->bhtd', p[:, :, :, j, :], vsel[j])
        return out

    out = lax.map(block, jnp.arange(S // Q_BLOCK))
    return jnp.moveaxis(out, 0, 2).reshape(B, H, S, Dh)


def even_mixer(h, w_in, w_out, a_re, a_im, b_re, b_im, c_re, c_im, d_skip, log_dt, glu_w):
    B, S, _ = h.shape
    proj = h @ w_in
    q, k, v, u = jnp.split(proj, [SB_WIDTH, 2 * SB_WIDTH, 3 * SB_WIDTH], axis=-1)
    heads = lambda t: t.reshape(B, S, SB_HEADS, HEAD_DIM).transpose(0, 2, 1, 3)
    o_a = stick_breaking_attention(heads(q), heads(k), heads(v))
    o_a = o_a.transpose(0, 2, 1, 3).reshape(B, S, SB_WIDTH)
    o_b = s5_mixer(u, a_re, a_im, b_re, b_im, c_re, c_im, d_skip, log_dt, glu_w)
    return jnp.concatenate([o_a, o_b], axis=-1) @ w_out


def odd_mixer(h, w_in, w_out, lb, norm_w):
    B, S, _ = h.shape
    proj = h @ w_in
    cuts = [HGRN_WIDTH, 2 * HGRN_WIDTH, 3 * HGRN_WIDTH, 4 * HGRN_WIDTH,
            4 * HGRN_WIDTH + MOBA_WIDTH, 4 * HGRN_WIDTH + 2 * MOBA_WIDTH]
    cq, cf, ci, cg, dq, dk, dv = jnp.split(proj, cuts, axis=-1)
    o_c = hgrn2_mixer(cq, cf, ci, cg, lb, norm_w)
    pos = jnp.arange(S)
    mh = lambda t: t.reshape(B, S, MOBA_HEADS, HEAD_DIM)
    qr = partial_rope(mh(dq), pos).transpose(0, 2, 1, 3)
    kr = partial_rope(mh(dk), pos).transpose(0, 2, 1, 3)
    vr = mh(dv).transpose(0, 2, 1, 3)
    o_d = moba_attention(qr, kr, vr).transpose(0, 2, 1, 3).reshape(B, S, MOBA_WIDTH)
    return jnp.concatenate([o_c, o_d.astype(o_c.dtype)], axis=-1) @ w_out


def setup_inputs(seed: int = 0) -> dict:
    key = jax.random.key(seed)
    ks = jax.random.split(key, 32)
    f32 = jnp.float32

    def nrm(k, shape, fan_in, gain=1.0):
        return jax.random.normal(k, shape, f32) * (gain * fan_in ** -0.5)

    x = jax.random.normal(ks[0], (BATCH, SEQ, D_MODEL), f32)
    c = jax.random.normal(ks[1], (BATCH, D_MODEL), f32)
    ada_w = nrm(ks[2], (D_MODEL, ADA_ROWS * D_MODEL), D_MODEL, 0.5)
    ada_table = 0.1 * jax.random.normal(ks[3], (DEPTH, ADA_ROWS, D_MODEL), f32)
    norm_pre = 1.0 + 0.05 * jax.random.normal(ks[4], (DEPTH, 3, D_MODEL), f32)
    norm_post = 1.0 + 0.05 * jax.random.normal(ks[5], (DEPTH, 3, D_MODEL), f32)
    ffn_w1 = nrm(ks[6], (DEPTH, 2, D_MODEL, D_FF), D_MODEL)
    ffn_w3 = nrm(ks[7], (DEPTH, 2, D_MODEL, D_FF), D_MODEL)
    ffn_w2 = nrm(ks[8], (DEPTH, 2, D_FF, D_MODEL), D_FF)
    ev_w_in = nrm(ks[9], (N_EVEN, D_MODEL, EVEN_IN), D_MODEL)
    ev_w_out = nrm(ks[10], (N_EVEN, MIX_WIDTH, D_MODEL), MIX_WIDTH)
    s5_a_re = -0.5 + 0.01 * jax.random.normal(ks[11], (N_EVEN, SSM_GROUPS, SSM_STATE), f32)
    s5_a_im = (math.pi * jnp.arange(SSM_STATE, dtype=f32)[None, None, :]
               + 0.01 * jax.random.normal(ks[12], (N_EVEN, SSM_GROUPS, SSM_STATE), f32))
    s5_b_re = nrm(ks[13], (N_EVEN, SSM_GROUPS, SSM_STATE, SSM_GROUP), 2 * SSM_GROUP)
    s5_b_im = nrm(ks[14], (N_EVEN, SSM_GROUPS, SSM_STATE, SSM_GROUP), 2 * SSM_GROUP)
    s5_c_re = nrm(ks[15], (N_EVEN, SSM_GROUPS, SSM_GROUP, SSM_STATE), SSM_STATE)
    s5_c_im = nrm(ks[16], (N_EVEN, SSM_GROUPS, SSM_GROUP, SSM_STATE), SSM_STATE)
    s5_d = jax.random.normal(ks[17], (N_EVEN, SSM_WIDTH), f32)
    s5_log_dt = jax.random.uniform(ks[18], (N_EVEN, SSM_GROUPS), f32,
                                   minval=math.log(SSM_DT_MIN), maxval=math.log(SSM_DT_MAX))
    s5_glu_w = nrm(ks[19], (N_EVEN, SSM_WIDTH, SSM_WIDTH), SSM_WIDTH)
    od_w_in = nrm(ks[20], (N_ODD, D_MODEL, ODD_IN), D_MODEL)
    od_w_out = nrm(ks[21], (N_ODD, MIX_WIDTH, D_MODEL), MIX_WIDTH)
    hgrn_lb = 0.5 * jax.random.normal(ks[22], (DEPTH, HGRN_WIDTH), f32)
    hgrn_norm_w = 1.0 + 0.05 * jax.random.normal(ks[23], (N_ODD, HEAD_DIM), f32)
    return {"x": x, "c": c, "ada_w": ada_w, "ada_table": ada_table,
            "norm_pre": norm_pre, "norm_post": norm_post,
            "ffn_w1": ffn_w1, "ffn_w3": ffn_w3, "ffn_w2": ffn_w2,
            "ev_w_in": ev_w_in, "ev_w_out": ev_w_out,
            "s5_a_re": s5_a_re, "s5_a_im": s5_a_im, "s5_b_re": s5_b_re, "s5_b_im": s5_b_im,
            "s5_c_re": s5_c_re, "s5_c_im": s5_c_im, "s5_d": s5_d, "s5_log_dt": s5_log_dt,
            "s5_glu_w": s5_glu_w, "od_w_in": od_w_in, "od_w_out": od_w_out,
            "hgrn_lb": hgrn_lb, "hgrn_norm_w": hgrn_norm_w}


def reference(x, c, ada_w, ada_table, norm_pre, norm_post, ffn_w1, ffn_w3, ffn_w2,
              ev_w_in, ev_w_out, s5_a_re, s5_a_im, s5_b_re, s5_b_im, s5_c_re, s5_c_im,
              s5_d, s5_log_dt, s5_glu_w, od_w_in, od_w_out, hgrn_lb, hgrn_norm_w):
    Bsz = x.shape[0]
    mod_shared = (jax.nn.silu(c) @ ada_w).reshape(Bsz, ADA_ROWS, D_MODEL)
    lb_cum = jnp.cumsum(jax.nn.softmax(hgrn_lb.astype(jnp.float32), axis=0), axis=0)
    lb_all = lb_cum - lb_cum[:1]
    for layer in range(DEPTH):
        mod = mod_shared + ada_table[layer][None]
        x = sandwich(x, lambda h: swiglu(h, ffn_w1[layer, 0], ffn_w3[layer, 0], ffn_w2[layer, 0]),
                     norm_pre[layer, 0], norm_post[layer, 0],
                     mod[:, 0], mod[:, 1], mod[:, 2], 0.5)
        if layer % 2 == 0:
            e = layer // 2
            mixer = lambda h: even_mixer(h, ev_w_in[e], ev_w_out[e], s5_a_re[e], s5_a_im[e],
                                         s5_b_re[e], s5_b_im[e], s5_c_re[e], s5_c_im[e],
                                         s5_d[e], s5_log_dt[e], s5_glu_w[e])
        else:
            o = layer // 2
            mixer = lambda h: odd_mixer(h, od_w_in[o], od_w_out[o], lb_all[layer], hgrn_norm_w[o])
        x = sandwich(x, mixer, norm_pre[layer, 1], norm_post[layer, 1],
                     mod[:, 3], mod[:, 4], mod[:, 5], 1.0)
        x = sandwich(x, lambda h: swiglu(h, ffn_w1[layer, 1], ffn_w3[layer, 1], ffn_w2[layer, 1]),
                     norm_pre[layer, 2], norm_post[layer, 2],
                     mod[:, 6], mod[:, 7], mod[:, 8], 0.5)
    return x
```

```python
import numpy as np
from contextlib import ExitStack
import concourse.bass as bass
import concourse.mybir as mybir
from concourse.bass_utils import run_bass_kernel_spmd

F32 = mybir.dt.float32
BF16 = mybir.dt.bfloat16
AF = mybir.ActivationFunctionType
ALU = mybir.AluOpType
AX = mybir.AxisListType

NCORES = 8
D_MODEL = 4096
SEQ = 8192
D_FF = 11008
RMS_EPS = 1e-6


class Buf:
    __slots__ = ("name", "w", "r")

    def __init__(self, name=""):
        self.name = name
        self.w = None
        self.r = []


class Prog:
    ENG = ("sync", "gpsimd", "scalar", "vector", "tensor")

    def __init__(self, nc, es, ndma=8):
        self.nc = nc
        _NC[0] = nc
        self.es = es
        self.q = {e: [] for e in self.ENG}
        self.cnt = {}
        self.sem = {}
        for e in ("gpsimd", "scalar", "vector", "tensor"):
            self.sem[e] = es.enter_context(nc.semaphore("s_" + e))
            self.cnt[e] = 0
        self.dsem, self.duse, self.dnext = {}, {}, {}
        for e in ("sync", "gpsimd"):
            self.dsem[e] = [es.enter_context(nc.semaphore("d_%s%d" % (e, i))) for i in range(ndma)]
            self.duse[e] = [0] * ndma
            self.dnext[e] = 0
        self.waited = {e: {} for e in self.ENG}
        self.semowner = {s.name: e for e, s in self.sem.items()}
        self.nid = 0

    def sb(self, shape, dtype, name=None):
        self.nid += 1
        return self.es.enter_context(self.nc.sbuf_tensor("S_%s_%d" % (name or "sb", self.nid), list(shape), dtype))

    def ps(self, shape, dtype=F32, name=None):
        self.nid += 1
        return self.es.enter_context(self.nc.psum_tensor("P_%s_%d" % (name or "ps", self.nid), list(shape), dtype))

    def _wait(self, eng, ev):
        sem, val = ev
        if self.semowner.get(sem.name) == eng and eng == "tensor":
            return
        w = self.waited[eng]
        if w.get(sem.name, 0) >= val:
            return
        w[sem.name] = val
        self.q[eng].append(lambda e, sem=sem, val=val: e.wait_ge(sem, val))

    def _deps(self, eng, reads, writes):
        for b in reads:
            if b.w is not None:
                self._wait(eng, b.w)
        for b in writes:
            if b.w is not None:
                self._wait(eng, b.w)
            for ev in b.r:
                self._wait(eng, ev)

    def _commit(self, ev, reads, writes):
        for b in reads:
            b.r.append(ev)
            if len(b.r) > 32:
                last = {}
                for s, v in b.r:
                    if s.name not in last or last[s.name][1] < v:
                        last[s.name] = (s, v)
                b.r = list(last.values())
        for b in writes:
            b.w = ev
            b.r = []

    def op(self, eng, fn, reads=(), writes=()):
        self._deps(eng, reads, writes)
        self.cnt[eng] += 1
        sem = self.sem[eng]
        ev = (sem, self.cnt[eng])
        self.q[eng].append(lambda e, fn=fn, sem=sem: fn(e).then_inc(sem, 1))
        self._commit(ev, reads, writes)
        return ev

    def dma(self, eng, out, in_, reads=(), writes=(), **kw):
        i = self.dnext[eng]
        self.dnext[eng] = (i + 1) % len(self.dsem[eng])
        sem = self.dsem[eng][i]
        if self.duse[eng][i] > 0:
            self._wait(eng, (sem, 16 * self.duse[eng][i]))
        self._deps(eng, reads, writes)
        self.duse[eng][i] += 1
        ev = (sem, 16 * self.duse[eng][i])
        self.q[eng].append(
            lambda e, out=out, in_=in_, sem=sem, kw=kw: e.dma_start(out=out, in_=in_, **kw).then_inc(sem, 16))
        self._commit(ev, reads, writes)
        return ev

    def all_events(self):
        evs = [(self.sem[e], self.cnt[e]) for e in self.sem if self.cnt[e] > 0]
        for e in self.dsem:
            for i, s in enumerate(self.dsem[e]):
                if self.duse[e][i] > 0:
                    evs.append((s, 16 * self.duse[e][i]))
        return evs

    def barrier(self):
        evs = self.all_events()
        for e in self.ENG:
            for ev in evs:
                self._wait(e, ev)

    def finish(self):
        for ev in self.all_events():
            self._wait("sync", ev)

    def scope(self):
        return _Scope(self)

    def emit(self):
        if not any(self.q[e] for e in self.ENG):
            return
        q = self.q
        self.q = {e: [] for e in self.ENG}
        self._emit_q(q)

    def _emit_q(self, q):
        self_q = q

        class _Q:
            pass
        holder = _Q()
        holder.q = self_q
        self = holder
        with _NC[0].Block() as block:
            @block.sync
            def _(e):
                for f in self.q["sync"]:
                    f(e)

            @block.gpsimd
            def _(e):
                for f in self.q["gpsimd"]:
                    f(e)

            @block.scalar
            def _(e):
                for f in self.q["scalar"]:
                    f(e)

            @block.vector
            def _(e):
                for f in self.q["vector"]:
                    f(e)

            @block.tensor
            def _(e):
                for f in self.q["tensor"]:
                    f(e)


class _Scope:
    def __init__(self, p):
        self.p = p

    def __enter__(self):
        self.saved = self.p.es
        self.st = ExitStack()
        self.st.__enter__()
        self.p.es = self.st
        return self

    def __exit__(self, *a):
        self.p.barrier()
        self.p.emit()
        self.p.es = self.saved
        return self.st.__exit__(*a)


_NC = [None]


class Rot:
    def __init__(self, items):
        self.items = items
        self.i = 0

    def next(self):
        it = self.items[self.i]
        self.i = (self.i + 1) % len(self.items)
        return it


def mk_rot(p, n, shape, dtype, psum=False, name="r"):
    items = []
    for i in range(n):
        t = p.ps(shape, dtype, name="%s_%d_%d" % (name, p.nid, i)) if psum else p.sb(shape, dtype, name="%s_%d_%d" % (name, p.nid, i))
        items.append((t, Buf(name)))
    return Rot(items)


def load_modvec(p, dram_ap, ncols, name):
    t = p.sb([128, ncols], F32, name=name)
    b = Buf(name)
    p.dma("sync", t[:], dram_ap, writes=[b])
    return t, b


def emit_rstd(p, ps_sum, b_sum, n, T, rstd, b_rstd):
    p.op("scalar", lambda e: e.activation(out=rstd[:, :T], in_=ps_sum[:, :T], func=AF.Sqrt, scale=1.0 / n, bias=RMS_EPS_AP[0][:, 0:1]),
         reads=[b_sum], writes=[b_rstd])
    p.op("vector", lambda e: e.reciprocal(out=rstd[:, :T], in_=rstd[:, :T]), reads=[b_rstd], writes=[b_rstd])


RMS_EPS_AP = [None]


def setup_consts(p):
    eps = p.sb([128, 1], F32, name="c_eps")
    ones = p.sb([128, 128], BF16, name="c_ones")
    b = Buf("consts")
    p.op("vector", lambda e: e.memset(eps[:], RMS_EPS), writes=[b])
    p.op("vector", lambda e: e.memset(ones[:], 1.0), writes=[b])
    RMS_EPS_AP[0] = eps
    return ones, b


def _sandwich_body(p, mode, DC, FC, TC, TT, res_w, T, NOUT=0, glu=False):
    D, FF = DC * 128, FC * 128
    NT = TC // TT
    GH = FC // 2
    xT_v = T["xT_v"]
    w1_v, w3_v, w2_v = T.get("w1_v"), T.get("w3_v"), T.get("w2_v")
    yT_v, oT_v, gluw_v, win_v, pT_v = T.get("yT_v"), T.get("oT_v"), T.get("gluw_v"), T.get("win_v"), T.get("pT_v")
    pT_chunk = T.get("pT_chunk") or (lambda m: pT_v[:, m, :])
    ones, b_const = setup_consts(p)
    vt = p.sb([128, 6 * DC], F32, name="vecs")
    gt = p.sb([128, 2 * DC], F32, name="gains")
    b_v, b_g = Buf("vecs"), Buf("gains")
    for (c0, ncol, ap) in T["vec_pieces"]:
        p.dma("sync", vt[:, c0:c0 + ncol], ap, writes=[b_v])
    for (c0, ncol, ap) in T["gain_pieces"]:
        p.dma("sync", gt[:, c0:c0 + ncol], ap, writes=[b_g])
    mv = p.sb([128, 3 * DC], F32, name="mv")
    b_mv = Buf("mv")
    A_, B_, CG = mv[:, 0:DC], mv[:, DC:2 * DC], mv[:, 2 * DC:3 * DC]
    p.op("vector", lambda e: e.tensor_tensor(out=B_, in0=vt[:, 0:DC], in1=vt[:, 3 * DC:4 * DC], op=ALU.add), reads=[b_v], writes=[b_mv])
    p.op("vector", lambda e: e.tensor_tensor(out=A_, in0=vt[:, DC:2 * DC], in1=vt[:, 4 * DC:5 * DC], op=ALU.add), reads=[b_v], writes=[b_mv])
    p.op("vector", lambda e: e.scalar_tensor_tensor(out=A_, in0=A_, scalar=1.0, in1=gt[:, 0:DC], op0=ALU.add, op1=ALU.mult), reads=[b_g, b_mv], writes=[b_mv])
    p.op("vector", lambda e: e.tensor_tensor(out=CG, in0=vt[:, 2 * DC:3 * DC], in1=vt[:, 5 * DC:6 * DC], op=ALU.add), reads=[b_v], writes=[b_mv])
    p.op("vector", lambda e: e.scalar_tensor_tensor(out=CG, in0=CG, scalar=float(res_w), in1=gt[:, DC:2 * DC], op0=ALU.mult, op1=ALU.mult), reads=[b_g, b_mv], writes=[b_mv])

    WN = 256
    KH = max(1, (FC + 1) // 2)
    RX = p.sb([128, max(DC * TT * 2, 4 * DC * WN)], BF16, name="RX")
    RH = p.sb([128, max(DC * TT, 2 * KH * 128)], BF16, name="RH")
    xs = RX[:, 0:DC * TT * 2].bitcast(F32).rearrange("p (c t) -> p c t", c=DC)
    ys = xs
    hT = RH[:, 0:DC * TT].rearrange("p (c t) -> p c t", c=DC)
    w13b = [RX[:, i * DC * WN:(i + 1) * DC * WN].rearrange("p (c n) -> p c n", c=DC) for i in range(4)]
    w2b = [RH[:, i * KH * 128:(i + 1) * KH * 128].rearrange("p (c n) -> p c n", c=KH) for i in range(2)]
    if mode in ("ffn", "out"):
        RG = p.sb([128, FC * TT], BF16, name="RG")
        gT = RG[:, :].rearrange("p (c t) -> p c t", c=FC)
    if mode == "out" and glu:
        g2t = p.sb([128, GH * TT], BF16, name="g2")
        g2 = g2t[:, :].rearrange("p (c t) -> p c t", c=GH)
    rstd = p.sb([128, TT], F32, name="rstd")
    b_rstd = Buf("rstd")
    tmp_r = mk_rot(p, 2, [128, TT], F32, name="tmp")
    sq_r = mk_rot(p, 2, [128, TT], BF16, name="sq")
    xst_r = mk_rot(p, 2, [128, TT], F32, name="xst")
    ost_r = mk_rot(p, 3, [128, TT], F32, name="ost")
    ps_sum = p.ps([128, 512], F32, name="ps_sum")
    b_pssum = Buf("ps_sum")
    psA = mk_rot(p, 2, [128, 512], F32, psum=True, name="psA")
    psB = mk_rot(p, 2, [128, 512], F32, psum=True, name="psB")

    for it in range(NT):
        tsl = slice(it * TT, (it + 1) * TT)
        b_x = [Buf("x%d" % c) for c in range(DC)]
        b_h = [Buf("h%d" % c) for c in range(DC)]
        b_G = [Buf("g%d" % c) for c in range(FC)]
        b_y = [Buf("y%d" % c) for c in range(DC)]
        if mode in ("ffn", "in"):
            for c in range(DC):
                p.dma("sync", xs[:, c, :], xT_v[:, c, tsl], writes=[b_x[c]])
            for c in range(DC):
                sq, b_sq = sq_r.next()
                p.op("scalar", lambda e, c=c, sq=sq: e.activation(out=sq[:], in_=xs[:, c, :], func=AF.Square), reads=[b_x[c]], writes=[b_sq])
                p.op("tensor", lambda e, c=c, sq=sq: e.matmul(ps_sum[:, :TT], ones[:], sq[:], start=(c == 0), stop=(c == DC - 1)),
                     reads=[b_sq, b_const], writes=[b_pssum])
            emit_rstd(p, ps_sum, b_pssum, D, TT, rstd, b_rstd)
            for c in range(DC):
                tmp, b_tmp = tmp_r.next()
                p.op("vector", lambda e, c=c, tmp=tmp: e.tensor_tensor(out=tmp[:], in0=xs[:, c, :], in1=rstd[:], op=ALU.mult), reads=[b_x[c], b_rstd], writes=[b_tmp])
                p.op("scalar", lambda e, c=c, tmp=tmp: e.activation(out=hT[:, c, :], in_=tmp[:], func=AF.Identity, scale=A_[:, c:c + 1], bias=B_[:, c:c + 1]),
                     reads=[b_tmp, b_mv], writes=[b_h[c]])
            p.barrier()
        if mode == "in":
            wbufs = Rot([(w13b[i], Buf("win%d" % i)) for i in range(4)])
            for iw in range(NOUT // WN):
                wa, b_w = wbufs.next()
                p.dma("gpsimd", wa, win_v[:, :, iw * WN:(iw + 1) * WN], writes=[b_w])
                for j in range(WN // 128):
                    m = iw * (WN // 128) + j
                    pa, b_pa = psA.next()
                    for k in range(DC):
                        p.op("tensor", lambda e, k=k, j=j, pa=pa, wa=wa: e.matmul(pa[:, :TT], wa[:, k, j * 128:(j + 1) * 128], hT[:, k, :], start=(k == 0), stop=(k == DC - 1)),
                             reads=[b_w, b_h[k]], writes=[b_pa])
                    ost, b_ost = ost_r.next()
                    p.op("scalar", lambda e, pa=pa, ost=ost: e.activation(out=ost[:], in_=pa[:, :TT], func=AF.Identity), reads=[b_pa], writes=[b_ost])
                    p.dma("sync", pT_chunk(m)[:, tsl], ost[:], reads=[b_ost])
            p.barrier()
            continue
        if mode == "ffn":
            nW = FF // WN
            wbufs = Rot([((w13b[0], w13b[1]), Buf("w13a")), ((w13b[2], w13b[3]), Buf("w13b"))])
            for iw in range(nW):
                (wa, wb_), b_w = wbufs.next()
                p.dma("gpsimd", wa, w1_v[:, :, iw * WN:(iw + 1) * WN], writes=[b_w])
                p.dma("gpsimd", wb_, w3_v[:, :, iw * WN:(iw + 1) * WN], writes=[b_w])
                for j in range(WN // 128):
                    m = iw * (WN // 128) + j
                    pa, b_pa = psA.next()
                    pb, b_pb = psB.next()
                    for k in range(DC):
                        p.op("tensor", lambda e, k=k, j=j, pa=pa, wa=wa: e.matmul(pa[:, :TT], wa[:, k, j * 128:(j + 1) * 128], hT[:, k, :], start=(k == 0), stop=(k == DC - 1)),
                             reads=[b_w, b_h[k]], writes=[b_pa])
                    for k in range(DC):
                        p.op("tensor", lambda e, k=k, j=j, pb=pb, wb_=wb_: e.matmul(pb[:, :TT], wb_[:, k, j * 128:(j + 1) * 128], hT[:, k, :], start=(k == 0), stop=(k == DC - 1)),
                             reads=[b_w, b_h[k]], writes=[b_pb])
                    tmp, b_tmp = tmp_r.next()
                    p.op("scalar", lambda e, pa=pa, tmp=tmp: e.activation(out=tmp[:], in_=pa[:, :TT], func=AF.Silu), reads=[b_pa], writes=[b_tmp])
                    p.op("vector", lambda e, pb=pb, tmp=tmp, m=m: e.tensor_tensor(out=gT[:, m, :], in0=tmp[:], in1=pb[:, :TT], op=ALU.mult), reads=[b_tmp, b_pb], writes=[b_G[m]])
            p.barrier()
        gsrc = [gT[:, k, :] for k in range(FC)] if mode in ("ffn", "out") else None
        if mode == "out":
            for c0 in range(0, FC, 8):
                for c in range(c0, min(FC, c0 + 8)):
                    p.dma("gpsimd", gT[:, c, :], oT_v[:, c, tsl], writes=[b_G[c]])
            if glu:
                b_G2 = [Buf("g2_%d" % c) for c in range(GH)]
                wbufs = Rot([(w13b[i][:, 0:GH, :], Buf("glu%d" % i)) for i in range(4)])
                for iw in range(GH * 128 // WN):
                    wa, b_w = wbufs.next()
                    p.dma("gpsimd", wa, gluw_v[:, :, iw * WN:(iw + 1) * WN], writes=[b_w])
                    for j in range(WN // 128):
                        m = iw * (WN // 128) + j
                        pa, b_pa = psA.next()
                        for k in range(GH):
                            p.op("tensor", lambda e, k=k, j=j, pa=pa, wa=wa: e.matmul(pa[:, :TT], wa[:, k, j * 128:(j + 1) * 128], gT[:, GH + k, :], start=(k == 0), stop=(k == GH - 1)),
                                 reads=[b_w, b_G[GH + k]], writes=[b_pa])
                        tmp, b_tmp = tmp_r.next()
                        p.op("scalar", lambda e, pa=pa, tmp=tmp: e.activation(out=tmp[:], in_=pa[:, :TT], func=AF.Sigmoid), reads=[b_pa], writes=[b_tmp])
                        p.op("vector", lambda e, tmp=tmp, m=m: e.tensor_tensor(out=g2[:, m, :], in0=tmp[:], in1=gT[:, GH + m, :], op=ALU.mult), reads=[b_tmp, b_G[GH + m]], writes=[b_G2[m]])
                for m in range(GH):
                    gsrc[GH + m] = g2[:, m, :]
                    b_G[GH + m] = b_G2[m]
            p.barrier()
        w2r = Rot([(w2b[0], Buf("w2a")), (w2b[1], Buf("w2b"))])
        for m in range(DC):
            pa, b_pa = psA.next()
            for half in range(2):
                k0 = half * KH
                k1 = min(FC, k0 + KH)
                wt, b_w = w2r.next()
                p.dma("gpsimd", wt[:, 0:k1 - k0, :], w2_v[:, k0:k1, m * 128:(m + 1) * 128], writes=[b_w])
                for k in range(k0, k1):
                    p.op("tensor", lambda e, k=k, k0=k0, pa=pa, wt=wt, g=gsrc[k]: e.matmul(pa[:, :TT], wt[:, k - k0, :], g, start=(k == 0), stop=(k == FC - 1)),
                         reads=[b_w, b_G[k]], writes=[b_pa])
            p.op("vector", lambda e, m=m, pa=pa: e.tensor_copy(out=ys[:, m, :], in_=pa[:, :TT]), reads=[b_pa], writes=[b_y[m]])
            sq, b_sq = sq_r.next()
            p.op("scalar", lambda e, m=m, sq=sq: e.activation(out=sq[:], in_=ys[:, m, :], func=AF.Square), reads=[b_y[m]], writes=[b_sq])
            p.op("tensor", lambda e, m=m, sq=sq: e.matmul(ps_sum[:, :TT], ones[:], sq[:], start=(m == 0), stop=(m == DC - 1)),
                 reads=[b_sq, b_const], writes=[b_pssum])
        emit_rstd(p, ps_sum, b_pssum, D, TT, rstd, b_rstd)
        for c in range(DC):
            xst, b_xst = xst_r.next()
            p.dma("sync", xst[:], xT_v[:, c, tsl], writes=[b_xst])
            tmp, b_tmp = tmp_r.next()
            p.op("vector", lambda e, c=c, tmp=tmp: e.tensor_tensor(out=tmp[:], in0=ys[:, c, :], in1=rstd[:], op=ALU.mult), reads=[b_y[c], b_rstd], writes=[b_tmp])
            ost, b_ost = ost_r.next()
            p.op("vector", lambda e, c=c, tmp=tmp, xst=xst, ost=ost: e.scalar_tensor_tensor(out=ost[:], in0=tmp[:], scalar=CG[:, c:c + 1], in1=xst[:], op0=ALU.mult, op1=ALU.add),
                 reads=[b_tmp, b_xst, b_mv], writes=[b_ost])
            p.dma("sync", yT_v[:, c, tsl], ost[:], reads=[b_ost])
        p.barrier()


def build_sandwich(mode, DC, FC, TC, TT, res_w, NOUT=0, glu=False):
    nc = bass.Bass("TRN2", target_bir_lowering=False)
    D, FF = DC * 128, FC * 128
    xT = nc.dram_tensor("xT", [D, TC], F32, kind="ExternalInput").ap()
    vecs = nc.dram_tensor("vecs", [128, 6 * DC], F32, kind="ExternalInput").ap()
    gains = nc.dram_tensor("gains", [128, 2 * DC], F32, kind="ExternalInput").ap()
    xT_v = xT.rearrange("(c p) t -> p c t", p=128)
    if mode == "ffn":
        w1 = nc.dram_tensor("w1", [D, FF], F32, kind="ExternalInput").ap()
        w3 = nc.dram_tensor("w3", [D, FF], F32, kind="ExternalInput").ap()
        w1_v = w1.rearrange("(c p) n -> p c n", p=128)
        w3_v = w3.rearrange("(c p) n -> p c n", p=128)
    if mode in ("ffn", "out"):
        w2 = nc.dram_tensor("w2", [FF, D], F32, kind="ExternalInput").ap()
        w2_v = w2.rearrange("(c p) n -> p c n", p=128)
        yT = nc.dram_tensor("yT", [D, TC], F32, kind="ExternalOutput").ap()
        yT_v = yT.rearrange("(c p) t -> p c t", p=128)
    if mode == "out":
        oT = nc.dram_tensor("oT", [FF, TC], F32, kind="ExternalInput").ap()
        oT_v = oT.rearrange("(c p) t -> p c t", p=128)
        if glu:
            GH = FC // 2
            gluw = nc.dram_tensor("gluw", [GH * 128, GH * 128], F32, kind="ExternalInput").ap()
            gluw_v = gluw.rearrange("(c p) n -> p c n", p=128)
    if mode == "in":
        win = nc.dram_tensor("win", [D, NOUT], F32, kind="ExternalInput").ap()
        win_v = win.rearrange("(c p) n -> p c n", p=128)
        pT = nc.dram_tensor("pT", [NOUT, TC], F32, kind="ExternalOutput").ap()
        pT_v = pT.rearrange("(c p) t -> p c t", p=128)
    NT = TC // TT
    T = {"xT_v": xT_v, "vec_pieces": [(0, 6 * DC, vecs)], "gain_pieces": [(0, 2 * DC, gains)]}
    for k_ in ("w1_v", "w3_v", "w2_v", "yT_v", "oT_v", "gluw_v", "win_v", "pT_v"):
        if k_ in locals():
            T[k_] = locals()[k_]
    with ExitStack() as es:
        p = Prog(nc, es)
        _sandwich_body(p, mode, DC, FC, TC, TT, res_w, T, NOUT=NOUT, glu=glu)
        p.finish()
        p.emit()
    return nc


def _mod_body(p, KC, NCH, cT, aw_v, modo):
    ct = p.sb([128, KC], F32, name="ct")
    sc = p.sb([128, KC], BF16, name="sc")
    b_c, b_s = Buf("c"), Buf("s")
    p.dma("sync", ct[:], cT, writes=[b_c])
    p.op("scalar", lambda e: e.activation(out=sc[:], in_=ct[:], func=AF.Silu), reads=[b_c], writes=[b_s])
    wr = mk_rot(p, 3, [128, KC, 256], BF16, name="aw")
    res = p.sb([128, NCH], F32, name="modres")
    b_r = Buf("r")
    GRP = 256
    for g0 in range(0, NCH, GRP):
        g1 = min(NCH, g0 + GRP)
        ps = p.ps([128, 512], F32, name="ps_mod")
        b_ps = Buf("psmod")
        for iw in range(g0 // 2, g1 // 2):
            wt, b_w = wr.next()
            p.dma("gpsimd", wt[:], aw_v[:, :, iw * 256:(iw + 1) * 256], writes=[b_w])
            for j in range(2):
                col = iw * 2 + j - g0
                for k in range(KC):
                    p.op("tensor", lambda e, k=k, j=j, wt=wt, col=col, ps=ps: e.matmul(ps[:, col:col + 1], wt[:, k, j * 128:(j + 1) * 128], sc[:, k:k + 1], start=(k == 0), stop=(k == KC - 1)),
                         reads=[b_w, b_s], writes=[b_ps])
        p.op("vector", lambda e, ps=ps, g0=g0, g1=g1: e.tensor_copy(out=res[:, g0:g1], in_=ps[:, 0:g1 - g0]), reads=[b_ps], writes=[b_r])
    p.dma("sync", modo, res[:], reads=[b_r])


def build_mod(KC, NCH):
    nc = bass.Bass("TRN2", target_bir_lowering=False)
    cT = nc.dram_tensor("cT", [128, KC], F32, kind="ExternalInput").ap()
    adaw = nc.dram_tensor("adaw", [KC * 128, NCH * 128], F32, kind="ExternalInput").ap()
    modo = nc.dram_tensor("modo", [128, NCH], F32, kind="ExternalOutput").ap()
    aw_v = adaw.rearrange("(c p) n -> p c n", p=128)
    with ExitStack() as es:
        p = Prog(nc, es)
        _mod_body(p, KC, NCH, cT, aw_v, modo)
        p.finish()
        p.emit()
    return nc


def load_cast_cols(p, dst, src, ncols, b, step=2048):
    for c0 in range(0, ncols, step):
        c1 = min(ncols, c0 + step)
        p.dma("gpsimd", dst[:, c0:c1], src[:, c0:c1], writes=[b])


def emit_sb_head(p, S, qT, b_q, kT, b_k, vb, b_v, consts, out_dram, TQ=512):
    tri, ones, masks, b_c = consts
    scale = 128 ** -0.5
    NB = TQ // 128
    zA = mk_rot(p, 2, [128, 512], F32, psum=True, name="zA")
    exB = mk_rot(p, 2, [128, 512], F32, psum=True, name="exB")
    csC = mk_rot(p, 1, [128, 512], F32, psum=True, name="csC")
    oD = p.ps([128, 512], F32, name="oD")
    b_oD = Buf("oD")
    e_r = mk_rot(p, 2, [128, TQ], F32, name="e")
    zs_r = mk_rot(p, 2, [128, TQ], F32, name="zs")
    sp_r = mk_rot(p, 2, [128, TQ], F32, name="sp")
    hi_r = mk_rot(p, 2, [128, TQ], BF16, name="hi")
    lo_r = mk_rot(p, 2, [128, TQ], BF16, name="lo")
    t_r = mk_rot(p, 2, [128, TQ], F32, name="t")
    w_r = mk_rot(p, 2, [128, TQ], BF16, name="w")
    o_r = mk_rot(p, 2, [128, TQ], F32, name="o")
    carry = p.sb([128, TQ], F32, name="carry%d" % p.nid)
    b_carry = Buf("carry")
    for qt in range(S // TQ):
        qs = slice(qt * TQ, (qt + 1) * TQ)
        p.op("vector", lambda e: e.memset(carry[:], 0.0), writes=[b_carry])
        kbs = list(range(qt * NB + NB - 1, -1, -1))
        for i, kb in enumerate(kbs):
            d = kb - qt * NB
            (za, b_za), (ex, b_ex), (cs, b_cs) = zA.next(), exB.next(), csC.next()
            (et, b_e), (zs, b_zs), (sp, b_sp) = e_r.next(), zs_r.next(), sp_r.next()
            (hi, b_hi), (lo, b_lo), (tt, b_t), (w, b_w) = hi_r.next(), lo_r.next(), t_r.next(), w_r.next()
            p.op("tensor", lambda e, za=za, kb=kb, qs=qs: e.matmul(za[:, :TQ], kT[:, kb * 128:(kb + 1) * 128], qT[:, qs], start=True, stop=True),
                 reads=[b_q, b_k], writes=[b_za])
            p.op("scalar", lambda e, za=za, et=et: e.activation(out=et[:], in_=za[:, :TQ], func=AF.Exp, scale=scale), reads=[b_za], writes=[b_e])
            p.op("scalar", lambda e, za=za, zs=zs: e.activation(out=zs[:], in_=za[:, :TQ], func=AF.Identity, scale=scale), reads=[b_za], writes=[b_zs])
            p.op("scalar", lambda e, et=et, sp=sp: e.activation(out=sp[:], in_=et[:], func=AF.Ln, bias=1.0, scale=1.0), reads=[b_e], writes=[b_sp])
            if d >= 0:
                p.op("vector", lambda e, sp=sp, d=d: e.tensor_tensor(out=sp[:], in0=sp[:], in1=masks[d], op=ALU.mult), reads=[b_sp, b_c], writes=[b_sp])
            p.op("vector", lambda e, sp=sp, hi=hi: e.tensor_copy(out=hi[:], in_=sp[:]), reads=[b_sp], writes=[b_hi])
            p.op("gpsimd", lambda e, sp=sp, hi=hi, lo=lo: e.tensor_tensor(out=lo[:], in0=sp[:], in1=hi[:], op=ALU.subtract), reads=[b_sp, b_hi], writes=[b_lo])
            p.op("gpsimd", lambda e, zs=zs, sp=sp, tt=tt: e.tensor_tensor(out=tt[:], in0=zs[:], in1=sp[:], op=ALU.subtract), reads=[b_zs, b_sp], writes=[b_t])
            p.op("tensor", lambda e, ex=ex, hi=hi: e.matmul(ex[:, :TQ], tri, hi[:], start=True, stop=False), reads=[b_hi, b_c], writes=[b_ex])
            p.op("tensor", lambda e, ex=ex, lo=lo: e.matmul(ex[:, :TQ], tri, lo[:], start=False, stop=True), reads=[b_lo, b_c], writes=[b_ex])
            p.op("tensor", lambda e, cs=cs, hi=hi: e.matmul(cs[:, :TQ], ones, hi[:], start=True, stop=False), reads=[b_hi, b_c], writes=[b_cs])
            p.op("tensor", lambda e, cs=cs, lo=lo: e.matmul(cs[:, :TQ], ones, lo[:], start=False, stop=True), reads=[b_lo, b_c], writes=[b_cs])
            p.op("vector", lambda e, tt=tt, ex=ex: e.tensor_tensor(out=tt[:], in0=tt[:], in1=ex[:, :TQ], op=ALU.subtract), reads=[b_t, b_ex], writes=[b_t])
            p.op("vector", lambda e, tt=tt: e.tensor_tensor(out=tt[:], in0=tt[:], in1=carry[:], op=ALU.subtract), reads=[b_t, b_carry], writes=[b_t])
            p.op("scalar", lambda e, tt=tt, w=w: e.activation(out=w[:], in_=tt[:], func=AF.Exp), reads=[b_t], writes=[b_w])
            if d >= 0:
                p.op("vector", lambda e, w=w, d=d: e.tensor_tensor(out=w[:], in0=w[:], in1=masks[d], op=ALU.mult), reads=[b_w, b_c], writes=[b_w])
            p.op("vector", lambda e, cs=cs: e.tensor_tensor(out=carry[:], in0=carry[:], in1=cs[:, :TQ], op=ALU.add), reads=[b_cs, b_carry], writes=[b_carry])
            p.op("tensor", lambda e, w=w, kb=kb, st=(i == 0), sp_=(i == len(kbs) - 1): e.matmul(oD[:, :TQ], vb[:, kb, :], w[:], start=st, stop=sp_),
                 reads=[b_w, b_v], writes=[b_oD])
        ot, b_o = o_r.next()
        p.op("scalar", lambda e, ot=ot: e.activation(out=ot[:], in_=oD[:, :TQ], func=AF.Identity), reads=[b_oD], writes=[b_o])
        p.dma("sync", out_dram[:, qs], ot[:], reads=[b_o])


def load_attn_consts(p, tri_d, ones_d, masks_d, TQ=512):
    b = Buf("aconst")
    tri = p.sb([128, 128], BF16, name="tri")
    ones = p.sb([128, 128], BF16, name="ones_a")
    NB = TQ // 128
    mk = p.sb([128, NB * TQ], F32, name="dmask")
    p.dma("gpsimd", tri[:], tri_d, writes=[b])
    p.dma("gpsimd", ones[:], ones_d, writes=[b])
    p.dma("sync", mk[:], masks_d, writes=[b])
    return (tri[:], ones[:], [mk[:, d * TQ:(d + 1) * TQ] for d in range(NB)], b)


def host_attn_consts(TQ=512, strict=True):
    j = np.arange(128)
    tri = (j[:, None] > j[None, :]).astype(np.float32)
    ones = np.ones((128, 128), np.float32)
    NB = TQ // 128
    q = np.arange(TQ)
    ms = []
    for d in range(NB):
        key = d * 128 + j
        ms.append((key[:, None] < q[None, :]) if strict else (key[:, None] <= q[None, :]))
    masks = np.concatenate(ms, axis=1).astype(np.float32)
    return tri, ones, masks


def build_sb_test(S, NH=1):
    nc = bass.Bass("TRN2", target_bir_lowering=False)
    qTd = nc.dram_tensor("qT", [NH, 128, S], F32, kind="ExternalInput").ap()
    kTd = nc.dram_tensor("kT", [NH, 128, S], F32, kind="ExternalInput").ap()
    vd = nc.dram_tensor("v", [NH, S, 128], F32, kind="ExternalInput").ap()
    trid = nc.dram_tensor("tri", [128, 128], F32, kind="ExternalInput").ap()
    onesd = nc.dram_tensor("ones", [128, 128], F32, kind="ExternalInput").ap()
    maskd = nc.dram_tensor("masks", [128, 4 * 512], F32, kind="ExternalInput").ap()
    oTd = nc.dram_tensor("oT", [NH, 128, S], F32, kind="ExternalOutput").ap()
    with ExitStack() as es:
        p = Prog(nc, es)
        consts = load_attn_consts(p, trid, onesd, maskd)
        emit_sb_all(p, S, NH, qTd, kTd, vd, consts, oTd)
        p.finish()
        p.emit()
    return nc


def emit_v_tokmajor(p, S, v_fm, vb, b_v, identb, b_id):
    with p.scope():
        vt = p.sb([128, S], BF16, name="vT_tmp")
        b_vt = Buf("vt")
        load_cast_cols(p, vt, v_fm, S, b_vt)
        tp = mk_rot(p, 2, [128, 1024], BF16, psum=True, name="vtp")
        for b0 in range(0, S // 128, 4):
            nb = min(4, S // 128 - b0)
            ps, b_ps = tp.next()
            for j in range(nb):
                p.op("tensor", lambda e, ps=ps, j=j, b0=b0: e.transpose(ps[:, j * 128:(j + 1) * 128], vt[:, (b0 + j) * 128:(b0 + j + 1) * 128], identb),
                     reads=[b_vt, b_id], writes=[b_ps])
            p.op("vector", lambda e, ps=ps, b0=b0, nb=nb: e.tensor_copy(out=vb[:, b0:b0 + nb, :], in_=ps[:, 0:nb * 128].rearrange("p (b d) -> p b d", d=128)),
                 reads=[b_ps], writes=[b_v])


def load_v(p, S, vb, b_v, v_tm=None, v_fm=None, identb=None, b_id=None):
    if v_tm is not None:
        vv = v_tm.rearrange("(b p) d -> p b d", p=128)
        for b0 in range(0, S // 128, 16):
            b1 = min(S // 128, b0 + 16)
            p.dma("gpsimd", vb[:, b0:b1, :], vv[:, b0:b1, :], writes=[b_v])
    else:
        emit_v_tokmajor(p, S, v_fm, vb, b_v, identb, b_id)


def emit_sb_heads(p, S, heads, consts, identb=None, b_id=None):
    for h, hd in enumerate(heads):
        with p.scope():
            qT = p.sb([128, S], BF16, name="qT_sb%d" % h)
            kT = p.sb([128, S], BF16, name="kT_sb%d" % h)
            vb = p.sb([128, S // 128, 128], BF16, name="v_sb%d" % h)
            b_q, b_k, b_v = Buf("q"), Buf("k"), Buf("v")
            load_cast_cols(p, qT, hd["q"], S, b_q)
            load_cast_cols(p, kT, hd["k"], S, b_k)
            load_v(p, S, vb, b_v, hd.get("v_tm"), hd.get("v_fm"), identb, b_id)
            emit_sb_head(p, S, qT, b_q, kT, b_k, vb, b_v, consts, hd["out"])


def emit_sb_all(p, S, NH, qTd, kTd, vd, consts, oTd):
    emit_sb_heads(p, S, [dict(q=qTd[h], k=kTd[h], v_tm=vd[h], out=oTd[h]) for h in range(NH)], consts)


ROPE_THETA = 500000.0
TWO_PI = 6.283185307179586
MOBA_NEG = -1e30


def host_moba_consts(S):
    NBLK = S // 256
    i = np.arange(32)
    inv = (ROPE_THETA ** (-(i % 16).astype(np.float32) / np.float32(16))).astype(np.float32)
    sgn = np.where(i < 16, -1.0, 1.0).astype(np.float32)
    ropec = np.stack([inv, sgn], axis=1).astype(np.float32)
    pos = np.broadcast_to(np.arange(S, dtype=np.float32)[None, :], (32, S)).copy()
    sel = np.zeros((128, NBLK, 128), np.float32)
    for b in range(NBLK):
        sel[b, b, :] = 1.0
    sel = sel.reshape(128, NBLK * 128)
    own = np.arange(NBLK)[:, None]
    b = np.arange(NBLK)[None, :]
    pastneg = np.where(b < own, 0.0, MOBA_NEG).astype(np.float32).reshape(1, NBLK * NBLK)
    owneye = (b == own).astype(np.float32).reshape(1, NBLK * NBLK)
    pastneg = np.broadcast_to(pastneg, (128, NBLK * NBLK)).copy()
    owneye = np.broadcast_to(owneye, (128, NBLK * NBLK)).copy()
    ident = np.eye(128, dtype=np.float32)
    tri, ones, masks = host_attn_consts(512, strict=False)
    return {"ropec": ropec, "pos": pos, "sel": sel, "pastneg": pastneg, "owneye": owneye, "ident": ident,
            "ones": ones, "masks": masks}


def emit_frac_sin(p, a, b_a, out, b_out, shape, tmps):
    ki, kf, t1 = tmps
    b_k, b_kf, b_t1 = Buf("ki"), Buf("kf"), Buf("t1")
    p.op("vector", lambda e: e.tensor_copy(out=ki, in_=a), reads=[b_a], writes=[b_k])
    p.op("vector", lambda e: e.tensor_copy(out=kf, in_=ki), reads=[b_k], writes=[b_kf])
    p.op("vector", lambda e: e.tensor_tensor(out=a, in0=a, in1=kf, op=ALU.subtract), reads=[b_a, b_kf], writes=[b_a])
    p.op("vector", lambda e: e.tensor_single_scalar(out=t1, in_=a, scalar=0.5, op=ALU.is_gt), reads=[b_a], writes=[b_t1])
    p.op("vector", lambda e: e.tensor_tensor(out=a, in0=a, in1=t1, op=ALU.subtract), reads=[b_a, b_t1], writes=[b_a])
    p.op("vector", lambda e: e.tensor_single_scalar(out=t1, in_=a, scalar=-0.5, op=ALU.is_lt), reads=[b_a], writes=[b_t1])
    p.op("vector", lambda e: e.tensor_tensor(out=a, in0=a, in1=t1, op=ALU.add), reads=[b_a, b_t1], writes=[b_a])
    p.op("scalar", lambda e: e.activation(out=out, in_=a, func=AF.Sin, scale=TWO_PI), reads=[b_a], writes=[b_out])


def emit_rope_rows(p, S, srcs, ropec_d, pos_d, dst_d, RP=2048):
    RP = min(RP, S)
    with p.scope():
        rc = p.sb([32, 2], F32, name="ropec")
        b_rc = Buf("rc")
        p.dma("sync", rc[:], ropec_d, writes=[b_rc])
        I32 = mybir.dt.int32
        pos = p.sb([32, RP], F32, name="pos")
        a = p.sb([32, RP], F32, name="ang")
        a2 = p.sb([32, RP], F32, name="ang2")
        ki = p.sb([32, RP], I32, name="ki")
        kf = p.sb([32, RP], F32, name="kf")
        t1 = p.sb([32, RP], F32, name="t1")
        Ct = p.sb([32, RP], F32, name="cos")
        St = p.sb([32, RP], F32, name="sin")
        x_r = mk_rot(p, 2, [32, RP], F32, name="x32")
        xs_r = mk_rot(p, 2, [32, RP], F32, name="xs32")
        b_pos, b_a, b_a2, b_C, b_S = Buf("pos"), Buf("a"), Buf("a2"), Buf("C"), Buf("S")
        for r0 in range(0, S, RP):
            p.dma("sync", pos[:], pos_d[:, r0:r0 + RP], writes=[b_pos])
            p.op("vector", lambda e: e.tensor_scalar(out=a[:], in0=pos[:], scalar1=rc[:, 0:1], scalar2=1.0 / TWO_PI, op0=ALU.mult, op1=ALU.mult),
                 reads=[b_pos, b_rc], writes=[b_a])
            p.op("vector", lambda e: e.tensor_single_scalar(out=a2[:], in_=a[:], scalar=0.25, op=ALU.add), reads=[b_a], writes=[b_a2])
            emit_frac_sin(p, a[:], b_a, St[:], b_S, [32, RP], (ki[:], kf[:], t1[:]))
            emit_frac_sin(p, a2[:], b_a2, Ct[:], b_C, [32, RP], (ki[:], kf[:], t1[:]))
            p.op("vector", lambda e: e.tensor_scalar(out=St[:], in0=St[:], scalar1=rc[:, 1:2], scalar2=None, op0=ALU.mult), reads=[b_S, b_rc], writes=[b_S])
            for i, (xd, xsd) in enumerate(srcs):
                x, b_x = x_r.next()
                xs, b_xs = xs_r.next()
                p.dma("sync", x[:], xd[:, r0:r0 + RP], writes=[b_x])
                p.dma("sync", xs[:], xsd[:, r0:r0 + RP], writes=[b_xs])
                p.op("vector", lambda e, x=x: e.tensor_tensor(out=x[:], in0=x[:], in1=Ct[:], op=ALU.mult), reads=[b_x, b_C], writes=[b_x])
                p.op("vector", lambda e, xs=xs: e.tensor_tensor(out=xs[:], in0=xs[:], in1=St[:], op=ALU.mult), reads=[b_xs, b_S], writes=[b_xs])
                p.op("vector", lambda e, x=x, xs=xs: e.tensor_tensor(out=x[:], in0=x[:], in1=xs[:], op=ALU.add), reads=[b_x, b_xs], writes=[b_x])
                p.dma("sync", dst_d[i, :, r0:r0 + RP], x[:], reads=[b_x])


def emit_moba_head(p, S, q_d, k_d, qr_d, kr_d, v_d, cst, out_d, TQ=512, v_fm=None, identb=None, b_id=None):
    NBLK = S // 256
    NQB = S // 128
    scale = 128 ** -0.5
    with p.scope():
        qf = p.sb([128, S], F32, name="qf")
        kf = p.sb([128, S], F32, name="kf")
        qb = p.sb([128, S], BF16, name="qb")
        kb_ = p.sb([128, S], BF16, name="kb")
        vb = p.sb([128, S // 128, 128], BF16, name="vb")
        biasT = p.sb([128, S], BF16, name="biasT")
        kmT = p.sb([128, NBLK], F32, name="kmT")
        b_qf, b_kf, b_qb, b_kb, b_v, b_bias, b_km = Buf("qf"), Buf("kf"), Buf("qb"), Buf("kb"), Buf("v"), Buf("bias"), Buf("km")
        p.dma("sync", qf[0:32, :], qr_d, writes=[b_qf])
        p.dma("sync", qf[32:128, :], q_d[32:128, :], writes=[b_qf])
        p.dma("sync", kf[0:32, :], kr_d, writes=[b_kf])
        p.dma("sync", kf[32:128, :], k_d[32:128, :], writes=[b_kf])
        load_v(p, S, vb, b_v, v_d, v_fm, identb, b_id)
        p.op("scalar", lambda e: e.activation(out=qb[:], in_=qf[:], func=AF.Identity), reads=[b_qf], writes=[b_qb])
        p.op("vector", lambda e: e.tensor_copy(out=kb_[:], in_=kf[:]), reads=[b_kf], writes=[b_kb])
        p.op("vector", lambda e: e.tensor_reduce(out=kmT[:], in_=kf[:].rearrange("p (b k) -> p b k", k=256), axis=AX.X, op=ALU.add), reads=[b_kf], writes=[b_km])
        p.op("vector", lambda e: e.tensor_single_scalar(out=kmT[:], in_=kmT[:], scalar=1.0 / 256, op=ALU.mult), reads=[b_km], writes=[b_km])
        p.op("vector", lambda e: e.memset(biasT[:], 0.0), writes=[b_bias])
        gps = mk_rot(p, 2, [128, NBLK], F32, psum=True, name="gps")
        tps = mk_rot(p, 2, [NBLK, 128], F32, psum=True, name="tps")
        gm_r = mk_rot(p, 2, [128, NBLK], F32, name="gm")
        t8_r = mk_rot(p, 2, [128, 8], F32, name="t8")
        sl_r = mk_rot(p, 2, [128, NBLK], F32, name="sl")
        for qbk in range(NQB):
            own = qbk // 2
            g, b_g = gps.next()
            tp, b_tp = tps.next()
            gm, b_gm = gm_r.next()
            t8, b_t8 = t8_r.next()
            sl, b_sl = sl_r.next()
            p.op("tensor", lambda e, g=g, qbk=qbk: e.matmul(g[:], qf[:, qbk * 128:(qbk + 1) * 128], kmT[:], start=True, stop=True), reads=[b_qf, b_km], writes=[b_g])
            p.op("vector", lambda e, g=g, gm=gm, own=own: e.tensor_tensor(out=gm[:], in0=g[:], in1=cst["pastneg"][:, own * NBLK:(own + 1) * NBLK], op=ALU.add), reads=[b_g, cst["b"]], writes=[b_gm])
            p.op("vector", lambda e, gm=gm, t8=t8: e.max(out=t8[:], in_=gm[:]), reads=[b_gm], writes=[b_t8])
            p.op("vector", lambda e, gm=gm, t8=t8, sl=sl: e.tensor_scalar(out=sl[:], in0=gm[:], scalar1=t8[:, 2:3], scalar2=None, op0=ALU.is_ge), reads=[b_gm, b_t8], writes=[b_sl])
            p.op("vector", lambda e, sl=sl, own=own: e.tensor_tensor(out=sl[:], in0=sl[:], in1=cst["owneye"][:, own * NBLK:(own + 1) * NBLK], op=ALU.max), reads=[b_sl, cst["b"]], writes=[b_sl])
            p.op("vector", lambda e, sl=sl: e.tensor_scalar(out=sl[:], in0=sl[:], scalar1=-1.0, scalar2=30000.0, op0=ALU.add, op1=ALU.mult), reads=[b_sl], writes=[b_sl])
            p.op("tensor", lambda e, tp=tp, sl=sl: e.transpose(tp[:], sl[:], cst["ident"]), reads=[b_sl, cst["b"]], writes=[b_tp])
            p.op("scalar", lambda e, tp=tp, qbk=qbk: e.activation(out=biasT[0:NBLK, qbk * 128:(qbk + 1) * 128], in_=tp[:], func=AF.Identity), reads=[b_tp], writes=[b_bias])
        NB = TQ // 128
        zA = mk_rot(p, 2, [128, 512], F32, psum=True, name="mzA")
        oD = p.ps([128, 512], F32, name="moD")
        sD = p.ps([128, 512], F32, name="msD")
        b_oD, b_sD = Buf("oD"), Buf("sD")
        P_r = mk_rot(p, 3, [128, TQ], BF16, name="P")
        rs_r = mk_rot(p, 2, [128, TQ], F32, name="rs")
        o_r = mk_rot(p, 2, [128, TQ], F32, name="mo")
        for qt in range(S // TQ):
            qs = slice(qt * TQ, (qt + 1) * TQ)
            kbs = list(range(qt * NB + NB - 1, -1, -1))
            for i, kb in enumerate(kbs):
                d = kb - qt * NB
                blk = kb // 2
                za, b_za = zA.next()
                P, b_P = P_r.next()
                p.op("tensor", lambda e, za=za, kb=kb, qs=qs: e.matmul(za[:, :TQ], kb_[:, kb * 128:(kb + 1) * 128], qb[:, qs], start=True, stop=False), reads=[b_qb, b_kb], writes=[b_za])
                p.op("tensor", lambda e, za=za, blk=blk, qs=qs: e.matmul(za[:, :TQ], cst["sel"][:, blk * 128:(blk + 1) * 128], biasT[:, qs], start=False, stop=True), reads=[b_bias, cst["b"]], writes=[b_za])
                p.op("scalar", lambda e, za=za, P=P: e.activation(out=P[:], in_=za[:, :TQ], func=AF.Exp, scale=scale), reads=[b_za], writes=[b_P])
                if d >= 0:
                    p.op("vector", lambda e, P=P, d=d: e.tensor_tensor(out=P[:], in0=P[:], in1=cst["masks"][d], op=ALU.mult), reads=[b_P, cst["b"]], writes=[b_P])
                st, sp_ = (i == 0), (i == len(kbs) - 1)
                p.op("tensor", lambda e, P=P, kb=kb, st=st, sp_=sp_: e.matmul(oD[:, :TQ], vb[:, kb, :], P[:], start=st, stop=sp_), reads=[b_P, b_v], writes=[b_oD])
                p.op("tensor", lambda e, P=P, st=st, sp_=sp_: e.matmul(sD[:, :TQ], cst["ones"], P[:], start=st, stop=sp_), reads=[b_P, cst["b"]], writes=[b_sD])
            rs, b_rs = rs_r.next()
            ot, b_o = o_r.next()
            p.op("vector", lambda e, rs=rs: e.reciprocal(out=rs[:], in_=sD[:, :TQ]), reads=[b_sD], writes=[b_rs])
            p.op("vector", lambda e, rs=rs, ot=ot: e.tensor_tensor(out=ot[:], in0=oD[:, :TQ], in1=rs[:], op=ALU.mult), reads=[b_oD, b_rs], writes=[b_o])
            p.dma("sync", out_d[:, qs], ot[:], reads=[b_o])


def load_moba_consts(p, d, S):
    NBLK = S // 256
    b = Buf("mconst")
    sel = p.sb([128, NBLK * 128], BF16, name="sel")
    ones = p.sb([128, 128], BF16, name="ones_m")
    pastneg = p.sb([128, NBLK * NBLK], F32, name="pastneg")
    owneye = p.sb([128, NBLK * NBLK], F32, name="owneye")
    ident = p.sb([128, 128], F32, name="ident")
    mk = p.sb([128, 4 * 512], F32, name="mmask")
    for c0 in range(0, NBLK * 128, 2048):
        c1 = min(NBLK * 128, c0 + 2048)
        p.dma("gpsimd", sel[:, c0:c1], d["sel"][:, c0:c1], writes=[b])
    p.dma("gpsimd", ones[:], d["ones"], writes=[b])
    p.dma("sync", pastneg[:], d["pastneg"], writes=[b])
    p.dma("sync", owneye[:], d["owneye"], writes=[b])
    p.dma("sync", ident[:], d["ident"], writes=[b])
    p.dma("sync", mk[:], d["masks"], writes=[b])
    return {"sel": sel, "ones": ones[:], "pastneg": pastneg, "owneye": owneye, "ident": ident[:],
            "masks": [mk[:, i * 512:(i + 1) * 512] for i in range(4)], "b": b}


def decl_moba_consts(nc, S):
    NBLK = S // 256
    shapes = {"ropec": [32, 2], "pos": [32, S], "sel": [128, NBLK * 128], "pastneg": [128, NBLK * NBLK], "owneye": [128, NBLK * NBLK],
              "ident": [128, 128], "ones": [128, 128], "masks": [128, 2048]}
    return {k: nc.dram_tensor("c_" + k, v, F32, kind="ExternalInput").ap() for k, v in shapes.items()}


def build_moba_test(S, NH=1):
    nc = bass.Bass("TRN2", target_bir_lowering=False)
    qd = nc.dram_tensor("qT", [NH, 128, S], F32, kind="ExternalInput").ap()
    kd = nc.dram_tensor("kT", [NH, 128, S], F32, kind="ExternalInput").ap()
    qsd = nc.dram_tensor("qsT", [NH, 32, S], F32, kind="ExternalInput").ap()
    ksd = nc.dram_tensor("ksT", [NH, 32, S], F32, kind="ExternalInput").ap()
    vd = nc.dram_tensor("v", [NH, S, 128], F32, kind="ExternalInput").ap()
    cd = decl_moba_consts(nc, S)
    rope_d = nc.dram_tensor("ropeo", [2 * NH, 32, S], F32, kind="ExternalOutput").ap()
    od = nc.dram_tensor("oT", [NH, 128, S], F32, kind="ExternalOutput").ap()
    with ExitStack() as es:
        p = Prog(nc, es)
        cst = load_moba_consts(p, cd, S)
        srcs = []
        for h in range(NH):
            srcs.append((qd[h, 0:32, :], qsd[h]))
            srcs.append((kd[h, 0:32, :], ksd[h]))
        emit_rope_rows(p, S, srcs, cd["ropec"], cd["pos"], rope_d)
        for h in range(NH):
            emit_moba_head(p, S, qd[h], kd[h], rope_d[2 * h], rope_d[2 * h + 1], vd[h], cst, od[h])
        p.finish()
        p.emit()
    return nc


def host_hgrn_consts():
    j = np.arange(128)
    return {"maskle": (j[:, None] <= j[None, :]).astype(np.float32), "ident": np.eye(128, dtype=np.float32),
            "ones": np.ones((128, 128), np.float32)}


def decl_hgrn_consts(nc):
    return {k: nc.dram_tensor("h_" + k, [128, 128], F32, kind="ExternalInput").ap() for k in ("maskle", "ident", "ones")}


def load_hgrn_consts(p, d):
    b = Buf("hconst")
    maskle = p.sb([128, 128], F32, name="maskle")
    identb = p.sb([128, 128], BF16, name="identb")
    onesb = p.sb([128, 128], BF16, name="onesb")
    onesf = p.sb([128, 128], F32, name="onesf")
    p.dma("sync", maskle[:], d["maskle"], writes=[b])
    maski = p.sb([128, 128], mybir.dt.int32, name="maski")
    p.op("vector", lambda e: e.tensor_copy(out=maski[:], in_=maskle[:]), reads=[b], writes=[b])
    p.dma("gpsimd", identb[:], d["ident"], writes=[b])
    p.dma("gpsimd", onesb[:], d["ones"], writes=[b])
    p.dma("sync", onesf[:], d["ones"], writes=[b])
    return {"maskle": maskle[:], "maski": maski[:], "identb": identb[:], "onesb": onesb[:], "onesf": onesf[:], "b": b}


def emit_hgrn_head(p, S, q_d, f_d, g_d, i_d, lb_d, nw_d, cst, out_d, i_fm=None):
    CH = 128
    MID = 63
    NCH = S // CH
    with p.scope():
        A = p.sb([128, S], BF16, name="hA")
        Bm = p.sb([128, S], BF16, name="hBm")
        EM = p.sb([128, NCH], F32, name="hEM")
        EL = p.sb([128, NCH], F32, name="hEL")
        lbt = p.sb([128, 4], F32, name="hlb")
        nw = p.sb([128, 1], F32, name="hnw")
        b_A, b_Bm, b_EM, b_EL, b_lb, b_nw = Buf("A"), Buf("Bm"), Buf("EM"), Buf("EL"), Buf("lb"), Buf("nw")
        p.dma("sync", lbt[:, 0:2], lb_d, writes=[b_lb])
        p.dma("sync", nw[:], nw_d, writes=[b_nw])
        p.op("vector", lambda e: e.tensor_tensor(out=lbt[:, 2:3], in0=lbt[:, 1:2], in1=lbt[:, 0:1], op=ALU.subtract), reads=[b_lb], writes=[b_lb])
        p.op("scalar", lambda e: e.activation(out=lbt[:, 2:3], in_=lbt[:, 2:3], func=AF.Sigmoid), reads=[b_lb], writes=[b_lb])
        p.op("vector", lambda e: e.tensor_scalar(out=lbt[:, 3:4], in0=lbt[:, 2:3], scalar1=-1.0, scalar2=1.0, op0=ALU.mult, op1=ALU.add), reads=[b_lb], writes=[b_lb])
        with p.scope():
            KK = p.sb([128, S], F32, name="hKK")
            G = p.sb([128, S], F32, name="hG")
            Q = p.sb([128, S], F32, name="hQ")
            E = p.sb([128, S], F32, name="hE")
            b_KK, b_G, b_Q, b_E = Buf("KK"), Buf("G"), Buf("Q"), Buf("E")
            p.dma("sync", KK[:], f_d, writes=[b_KK])
            p.dma("sync", Q[:], q_d, writes=[b_Q])
            p.op("scalar", lambda e: e.activation(out=KK[:], in_=KK[:], func=AF.Sigmoid), reads=[b_KK], writes=[b_KK])
            p.op("vector", lambda e: e.tensor_scalar(out=KK[:], in0=KK[:], scalar1=lbt[:, 3:4], scalar2=lbt[:, 2:3], op0=ALU.mult, op1=ALU.add), reads=[b_KK, b_lb], writes=[b_KK])
            p.op("scalar", lambda e: e.activation(out=E[:], in_=KK[:], func=AF.Ln), reads=[b_KK], writes=[b_E])
            p.op("vector", lambda e: e.tensor_scalar(out=KK[:], in0=KK[:], scalar1=-1.0, scalar2=1.0, op0=ALU.mult, op1=ALU.add), reads=[b_KK], writes=[b_KK])
            for c in range(NCH):
                cs = slice(c * CH, (c + 1) * CH)
                p.op("vector", lambda e, cs=cs: e.tensor_tensor_scan(out=G[:, cs], data0=cst["onesf"], data1=E[:, cs], initial=0.0, op0=ALU.mult, op1=ALU.add),
                     reads=[b_E, cst["b"]], writes=[b_G])
            G3 = G[:, :].rearrange("p (c t) -> p c t", t=CH)
            p.op("scalar", lambda e: e.activation(out=EM[:], in_=G3[:, :, MID], func=AF.Exp), reads=[b_G], writes=[b_EM])
            Gm = p.sb([128, NCH], F32, name="hGm")
            b_Gm = Buf("Gm")
            p.op("vector", lambda e: e.tensor_copy(out=Gm[:], in_=G3[:, :, MID]), reads=[b_G], writes=[b_Gm])
            for c in range(NCH):
                cs = slice(c * CH, (c + 1) * CH)
                p.op("vector", lambda e, cs=cs, c=c: e.tensor_scalar(out=G[:, cs], in0=G[:, cs], scalar1=Gm[:, c:c + 1], scalar2=None, op0=ALU.subtract), reads=[b_G, b_Gm], writes=[b_G])
            p.op("scalar", lambda e: e.activation(out=EL[:], in_=G3[:, :, CH - 1], func=AF.Exp), reads=[b_G], writes=[b_EL])
            p.op("scalar", lambda e: e.activation(out=E[:], in_=G[:], func=AF.Exp), reads=[b_G], writes=[b_E])
            p.op("vector", lambda e: e.tensor_tensor(out=A[:], in0=Q[:], in1=E[:], op=ALU.mult), reads=[b_Q, b_E], writes=[b_A])
            p.op("scalar", lambda e: e.activation(out=E[:], in_=G[:], func=AF.Exp, scale=-1.0), reads=[b_G, b_A], writes=[b_E])
            p.op("vector", lambda e: e.tensor_tensor(out=Bm[:], in0=KK[:], in1=E[:], op=ALU.mult), reads=[b_KK, b_E], writes=[b_Bm])
        oT = p.sb([128, S], F32, name="hoT")
        b_oT = Buf("oT")
        with p.scope():
            vb = p.sb([128, NCH, 128], BF16, name="hvb")
            b_v = Buf("v")
            load_v(p, S, vb, b_v, i_d, i_fm, cst["identb"], cst["b"])
            state = p.sb([128, 128], F32, name="hstate")
            b_st = Buf("state")
            p.op("vector", lambda e: e.memset(state[:], 0.0), writes=[b_st])
            att_ps = mk_rot(p, 2, [128, 512], F32, psum=True, name="hatt")
            o_ps = mk_rot(p, 2, [128, 512], F32, psum=True, name="hops")
            bt_ps = mk_rot(p, 2, [128, 1024], BF16, psum=True, name="hbt")
            ds_ps = mk_rot(p, 2, [128, 512], F32, psum=True, name="hds")
            attm_r = mk_rot(p, 2, [128, 128], BF16, name="hattm")
            for (t_, b_) in attm_r.items:
                p.op("vector", lambda e, t_=t_: e.memset(t_[:], 0.0), writes=[b_])
            sm_r = mk_rot(p, 2, [128, 128], BF16, name="hsm")
            bt_r = mk_rot(p, 2, [128, 128], BF16, name="hbtsb")
            tmp_r = mk_rot(p, 2, [128, 128], F32, name="htmp")
            for c in range(NCH):
                cs = slice(c * CH, (c + 1) * CH)
                (at, b_at), (op_, b_op), (bt, b_bt), (ds, b_ds) = att_ps.next(), o_ps.next(), bt_ps.next(), ds_ps.next()
                (attm, b_attm), (sm, b_sm), (bts, b_bts), (tmp, b_tmp) = attm_r.next(), sm_r.next(), bt_r.next(), tmp_r.next()
                p.op("tensor", lambda e, at=at, cs=cs: e.matmul(at[:, 0:128], Bm[:, cs], A[:, cs], start=True, stop=True), reads=[b_A, b_Bm], writes=[b_at])
                p.op("vector", lambda e, at=at, attm=attm: e.copy_predicated(out=attm[:], mask=cst["maski"], data=at[:, 0:128]), reads=[b_at, cst["b"]], writes=[b_attm])
                p.op("scalar", lambda e, sm=sm, c=c: e.activation(out=sm[:], in_=state[:], func=AF.Identity, scale=EM[:, c:c + 1]), reads=[b_st, b_EM], writes=[b_sm])
                p.op("tensor", lambda e, op_=op_, attm=attm, c=c: e.matmul(op_[:, 0:128], vb[:, c, :], attm[:], start=True, stop=False), reads=[b_v, b_attm], writes=[b_op])
                p.op("tensor", lambda e, op_=op_, sm=sm, cs=cs: e.matmul(op_[:, 0:128], sm[:], A[:, cs], start=False, stop=True), reads=[b_sm, b_A], writes=[b_op])
                p.op("scalar", lambda e, op_=op_, cs=cs: e.activation(out=oT[:, cs], in_=op_[:, 0:128], func=AF.Identity), reads=[b_op], writes=[b_oT])
                p.op("tensor", lambda e, bt=bt, cs=cs: e.transpose(bt[:, 0:128], Bm[:, cs], cst["identb"]), reads=[b_Bm, cst["b"]], writes=[b_bt])
                p.op("vector", lambda e, bt=bt, bts=bts: e.tensor_copy(out=bts[:], in_=bt[:, 0:128]), reads=[b_bt], writes=[b_bts])
                p.op("tensor", lambda e, ds=ds, bts=bts, c=c: e.matmul(ds[:, 0:128], bts[:], vb[:, c, :], start=True, stop=True), reads=[b_bts, b_v], writes=[b_ds])
                p.op("vector", lambda e, ds=ds, tmp=tmp, c=c: e.scalar_tensor_tensor(out=tmp[:], in0=state[:], scalar=EM[:, c:c + 1], in1=ds[:, 0:128], op0=ALU.mult, op1=ALU.add),
                     reads=[b_st, b_EM, b_ds], writes=[b_tmp])
                p.op("vector", lambda e, tmp=tmp, c=c: e.tensor_scalar(out=state[:], in0=tmp[:], scalar1=EL[:, c:c + 1], scalar2=None, op0=ALU.mult), reads=[b_tmp, b_EL], writes=[b_st])
        with p.scope():
            Gt = p.sb([128, S], F32, name="hgate")
            b_Gt = Buf("gate")
            p.dma("sync", Gt[:], g_d, writes=[b_Gt])
            eps = p.sb([128, 1], F32, name="heps")
            b_eps = Buf("eps")
            p.op("vector", lambda e: e.memset(eps[:], RMS_EPS), writes=[b_eps])
            TT = min(512, S)
            ssum = mk_rot(p, 2, [128, 512], F32, psum=True, name="hss")
            sq_r = mk_rot(p, 2, [128, TT], BF16, name="hsq")
            r_r = mk_rot(p, 2, [128, TT], F32, name="hr")
            t_r = mk_rot(p, 2, [128, TT], F32, name="ht")
            for t0 in range(0, S, TT):
                ts_ = slice(t0, t0 + TT)
                (ss, b_ss), (sq, b_sq), (r, b_r), (t, b_t) = ssum.next(), sq_r.next(), r_r.next(), t_r.next()
                p.op("scalar", lambda e, sq=sq, ts_=ts_: e.activation(out=sq[:], in_=oT[:, ts_], func=AF.Square), reads=[b_oT], writes=[b_sq])
                p.op("tensor", lambda e, ss=ss, sq=sq: e.matmul(ss[:, :TT], cst["onesb"], sq[:], start=True, stop=True), reads=[b_sq, cst["b"]], writes=[b_ss])
                p.op("scalar", lambda e, ss=ss, r=r: e.activation(out=r[:], in_=ss[:, :TT], func=AF.Sqrt, scale=1.0 / 128, bias=eps[:, 0:1]), reads=[b_ss, b_eps], writes=[b_r])
                p.op("vector", lambda e, r=r: e.reciprocal(out=r[:], in_=r[:]), reads=[b_r], writes=[b_r])
                p.op("scalar", lambda e, ts_=ts_: e.activation(out=Gt[:, ts_], in_=Gt[:, ts_], func=AF.Silu), reads=[b_Gt], writes=[b_Gt])
                p.op("vector", lambda e, r=r, t=t, ts_=ts_: e.tensor_tensor(out=t[:], in0=oT[:, ts_], in1=r[:], op=ALU.mult), reads=[b_oT, b_r], writes=[b_t])
                p.op("vector", lambda e, t=t, ts_=ts_: e.scalar_tensor_tensor(out=t[:], in0=t[:], scalar=nw[:, 0:1], in1=Gt[:, ts_], op0=ALU.mult, op1=ALU.mult),
                     reads=[b_t, b_nw, b_Gt], writes=[b_t])
                p.dma("sync", out_d[:, ts_], t[:], reads=[b_t])


def build_hgrn_test(S):
    nc = bass.Bass("TRN2", target_bir_lowering=False)
    qd = nc.dram_tensor("qT", [128, S], F32, kind="ExternalInput").ap()
    fd = nc.dram_tensor("fT", [128, S], F32, kind="ExternalInput").ap()
    gd = nc.dram_tensor("gT", [128, S], F32, kind="ExternalInput").ap()
    idd = nc.dram_tensor("itm", [S, 128], F32, kind="ExternalInput").ap()
    lbd = nc.dram_tensor("lb", [128, 2], F32, kind="ExternalInput").ap()
    nwd = nc.dram_tensor("nw", [128, 1], F32, kind="ExternalInput").ap()
    cd = decl_hgrn_consts(nc)
    od = nc.dram_tensor("oT", [128, S], F32, kind="ExternalOutput").ap()
    with ExitStack() as es:
        p = Prog(nc, es)
        cst = load_hgrn_consts(p, cd)
        emit_hgrn_head(p, S, qd, fd, gd, idd, lbd, nwd, cst, od)
        p.finish()
        p.emit()
    return nc


S5_LC = 512


def host_s5_consts(Lc=S5_LC):
    iota = np.broadcast_to(np.arange(Lc, dtype=np.float32)[None, :], (128, Lc)).copy()
    sgn2 = np.where(np.arange(128) < 64, 1.0, -1.0).astype(np.float32)[:, None]
    swapP = np.zeros((128, 128), np.float32)
    for m in range(128):
        swapP[(m + 64) % 128, m] = 1.0
    rowmask = (np.arange(128)[:, None] // 16 == np.arange(8)[None, :]).astype(np.float32)
    ones = np.ones((128, Lc), np.float32)
    return {"iota": iota, "sgn2": sgn2, "swapP": swapP, "rowmask": rowmask, "onesL": ones}


def host_s5_params(a_re, a_im, b_re, b_im, c_re, c_im, d_skip, log_dt, g0, ng=8):
    gs = slice(g0, g0 + ng)
    rep = lambda a: np.repeat(a[gs], 16, axis=0).astype(np.float32)
    arep, airep = rep(a_re), rep(a_im)
    dtrep = np.repeat(log_dt[gs], 16)[:, None].astype(np.float32)
    bT = lambda b: np.ascontiguousarray(b[gs].transpose(0, 2, 1).reshape(ng * 16, 64)).astype(np.float32)
    a2 = lambda a: np.ascontiguousarray(np.concatenate([a[gs].T, a[gs].T], axis=0)).astype(np.float32)
    dt2 = np.broadcast_to(log_dt[gs][None, :], (128, ng)).astype(np.float32).copy()
    cT = np.concatenate([c_re[gs].transpose(2, 0, 1), c_im[gs].transpose(2, 0, 1)], axis=0)
    cT = np.ascontiguousarray(cT.reshape(128, ng * 16)).astype(np.float32)
    d2 = d_skip[g0 * 16:(g0 + ng) * 16][:, None].astype(np.float32)
    return {"arep": arep, "airep": airep, "dtrep": dtrep, "bTre": bT(b_re), "bTim": bT(b_im),
            "a2re": a2(a_re), "a2im": a2(a_im), "dt2": dt2, "cT": cT, "d2": d2}


S5_PARAM_SHAPES = {"arep": [128, 64], "airep": [128, 64], "dtrep": [128, 1], "bTre": [128, 64], "bTim": [128, 64],
                   "a2re": [128, 8], "a2im": [128, 8], "dt2": [128, 8], "cT": [128, 128], "d2": [128, 1]}


def decl_s5(nc, nck, Lc=S5_LC):
    cs = {"iota": [128, Lc], "sgn2": [128, 1], "swapP": [128, 128], "rowmask": [128, 8], "onesL": [128, Lc]}
    cd = {k: nc.dram_tensor("s_" + k, v, F32, kind="ExternalInput").ap() for k, v in cs.items()}
    pd = [{k: nc.dram_tensor("s%d_%s" % (i, k), v, F32, kind="ExternalInput").ap() for k, v in S5_PARAM_SHAPES.items()} for i in range(nck)]
    return cd, pd


def emit_sincos_small(p, turns, b_t, n, name):
    I32 = mybir.dt.int32
    a = p.sb([128, n], F32, name=name + "a")
    ki = p.sb([128, n], I32, name=name + "ki")
    kf = p.sb([128, n], F32, name=name + "kf")
    t1 = p.sb([128, n], F32, name=name + "t1")
    sn = p.sb([128, n], F32, name=name + "sin")
    cs = p.sb([128, n], F32, name=name + "cos")
    b_a, b_sn, b_cs = Buf("a"), Buf("sn"), Buf("cs")
    p.op("vector", lambda e: e.tensor_copy(out=a[:], in_=turns), reads=[b_t], writes=[b_a])
    emit_frac_sin(p, a[:], b_a, sn[:], b_sn, None, (ki[:], kf[:], t1[:]))
    p.op("vector", lambda e: e.tensor_single_scalar(out=a[:], in_=turns, scalar=0.25, op=ALU.add), reads=[b_t, b_sn], writes=[b_a])
    emit_frac_sin(p, a[:], b_a, cs[:], b_cs, None, (ki[:], kf[:], t1[:]))
    return sn, b_sn, cs, b_cs


def emit_s5_chunk(p, S, u_d, cd, pd, out_d, Lc=S5_LC):
    NG = 8
    Lc = min(Lc, S)
    NTC = S // Lc
    I32 = mybir.dt.int32
    inv2pi = 1.0 / TWO_PI
    with p.scope():
        b_c = Buf("s5c")
        cst = {}
        for k, shp in (("iota", [128, Lc]), ("sgn2", [128, 1]), ("swapP", [128, 128]), ("rowmask", [128, 8]), ("onesL", [128, Lc])):
            cst[k] = p.sb(shp, F32, name="s5" + k)
            p.dma("sync", cst[k][:], cd[k], writes=[b_c])
        prm = {}
        b_p = Buf("s5p")
        for k, shp in S5_PARAM_SHAPES.items():
            prm[k] = p.sb(shp, F32, name="s5p" + k)
            p.dma("sync", prm[k][:], pd[k], writes=[b_p])
        WxA = p.sb([128, NG, 128], BF16, name="WxA")
        WxB = p.sb([128, NG, 128], BF16, name="WxB")
        Cpad = p.sb([128, NG, 128], BF16, name="Cpad")
        b_W, b_C = Buf("Wx"), Buf("Cpad")
        rho = p.sb([128, NG], F32, name="rho")
        cL = p.sb([128, NG], F32, name="cL")
        nsL = p.sb([128, NG], F32, name="nsL")
        b_rho, b_cL = Buf("rho"), Buf("cL")
        Ct = p.sb([128, NG, Lc], F32, name="Ct")
        Sg = p.sb([128, NG, Lc], F32, name="Sg")
        Rt = p.sb([128, NG, Lc], F32, name="Rt")
        b_tab = Buf("tab")
        with p.scope():
            V = lambda name, n=64: p.sb([128, n], F32, name="s5v" + name)
            dt = V("dt", 1)
            b1 = Buf("b1")
            p.op("scalar", lambda e: e.activation(out=dt[:], in_=prm["dtrep"][:], func=AF.Exp), reads=[b_p], writes=[b1])
            mag, turns = V("mag"), V("turns")
            b_mag, b_tu = Buf("mag"), Buf("tu")
            p.op("scalar", lambda e: e.activation(out=mag[:], in_=prm["arep"][:], func=AF.Exp, scale=dt[:, 0:1]), reads=[b_p, b1], writes=[b_mag])
            p.op("vector", lambda e: e.tensor_scalar(out=turns[:], in0=prm["airep"][:], scalar1=dt[:, 0:1], scalar2=inv2pi, op0=ALU.mult, op1=ALU.mult), reads=[b_p, b1], writes=[b_tu])
            sn, b_sn, cs, b_cs = emit_sincos_small(p, turns[:], b_tu, 64, "w")
            lr, li, den, t2, cr, ci, t3 = V("lr"), V("li"), V("den"), V("t2"), V("cr"), V("ci"), V("t3")
            bx = Buf("bx")
            R_ = [b_p, b_mag, b_sn, b_cs, bx]
            ar, ai, br, bi = prm["arep"], prm["airep"], prm["bTre"], prm["bTim"]
            TT_ = lambda o, a, b, op: p.op("vector", lambda e: e.tensor_tensor(out=o, in0=a, in1=b, op=op), reads=R_, writes=[bx])
            TT_(lr[:], mag[:], cs[:], ALU.mult)
            TT_(li[:], mag[:], sn[:], ALU.mult)
            TT_(den[:], ar[:], ar[:], ALU.mult)
            TT_(t2[:], ai[:], ai[:], ALU.mult)
            TT_(den[:], den[:], t2[:], ALU.add)
            p.op("vector", lambda e: e.reciprocal(out=den[:], in_=den[:]), reads=R_, writes=[bx])
            p.op("vector", lambda e: e.tensor_single_scalar(out=lr[:], in_=lr[:], scalar=-1.0, op=ALU.add), reads=R_, writes=[bx])
            TT_(cr[:], lr[:], ar[:], ALU.mult)
            TT_(t2[:], li[:], ai[:], ALU.mult)
            TT_(cr[:], cr[:], t2[:], ALU.add)
            TT_(cr[:], cr[:], den[:], ALU.mult)
            TT_(ci[:], li[:], ar[:], ALU.mult)
            TT_(t2[:], lr[:], ai[:], ALU.mult)
            TT_(ci[:], ci[:], t2[:], ALU.subtract)
            TT_(ci[:], ci[:], den[:], ALU.mult)
            WA = p.sb([128, 128], F32, name="s5WA")
            WB = p.sb([128, 128], F32, name="s5WB")
            TT_(t2[:], cr[:], br[:], ALU.mult)
            TT_(t3[:], ci[:], bi[:], ALU.mult)
            TT_(WA[:, 0:64], t2[:], t3[:], ALU.subtract)
            TT_(t2[:], cr[:], bi[:], ALU.mult)
            TT_(t3[:], ci[:], br[:], ALU.mult)
            TT_(WA[:, 64:128], t2[:], t3[:], ALU.add)
            p.op("vector", lambda e: e.tensor_copy(out=WB[:, 0:64], in_=WA[:, 64:128]), reads=R_, writes=[bx])
            p.op("vector", lambda e: e.tensor_copy(out=WB[:, 64:128], in_=WA[:, 0:64]), reads=R_, writes=[bx])
            for gl in range(NG):
                p.op("vector", lambda e, gl=gl: e.tensor_scalar(out=WxA[:, gl, :], in0=WA[:], scalar1=cst["rowmask"][:, gl:gl + 1], scalar2=None, op0=ALU.mult), reads=[bx, b_c], writes=[b_W])
                p.op("vector", lambda e, gl=gl: e.tensor_scalar(out=WxB[:, gl, :], in0=WB[:], scalar1=cst["rowmask"][:, gl:gl + 1], scalar2=None, op0=ALU.mult), reads=[bx, b_c], writes=[b_W])
            p.op("vector", lambda e: e.memset(Cpad[:], 0.0), writes=[b_C])
            for gl in range(NG):
                p.op("vector", lambda e, gl=gl: e.tensor_scalar(out=Cpad[:, gl, 16 * gl:16 * gl + 16], in0=prm["cT"][:, 16 * gl:16 * gl + 16], scalar1=cst["sgn2"][:, 0:1], scalar2=None, op0=ALU.mult),
                     reads=[b_p, b_c, b_C], writes=[b_C])
            dt8, th = V("dt8", NG), V("th", NG)
            b2 = Buf("b2")
            p.op("scalar", lambda e: e.activation(out=dt8[:], in_=prm["dt2"][:], func=AF.Exp), reads=[b_p], writes=[b2])
            p.op("vector", lambda e: e.tensor_tensor(out=rho[:], in0=prm["a2re"][:], in1=dt8[:], op=ALU.mult), reads=[b_p, b2], writes=[b_rho])
            p.op("scalar", lambda e: e.activation(out=rho[:], in_=rho[:], func=AF.Exp), reads=[b_rho], writes=[b_rho])
            p.op("vector", lambda e: e.scalar_tensor_tensor(out=th[:], in0=prm["a2im"][:], scalar=inv2pi, in1=dt8[:], op0=ALU.mult, op1=ALU.mult), reads=[b_p, b2], writes=[b2])
            thL = V("thL", NG)
            b3 = Buf("b3")
            p.op("vector", lambda e: e.tensor_single_scalar(out=thL[:], in_=th[:], scalar=float(Lc), op=ALU.mult), reads=[b2], writes=[b3])
            snL, b_snL, csL, b_csL = emit_sincos_small(p, thL[:], b3, NG, "L")
            p.op("vector", lambda e: e.tensor_copy(out=cL[:], in_=csL[:]), reads=[b_csL], writes=[b_cL])
            p.op("vector", lambda e: e.tensor_scalar(out=nsL[:], in0=snL[:], scalar1=cst["sgn2"][:, 0:1], scalar2=-1.0, op0=ALU.mult, op1=ALU.mult), reads=[b_snL, b_c], writes=[b_cL])
            a = p.sb([128, Lc], F32, name="s5ta")
            ki = p.sb([128, Lc], I32, name="s5tki")
            kf = p.sb([128, Lc], F32, name="s5tkf")
            t1 = p.sb([128, Lc], F32, name="s5tt1")
            b_a = Buf("ta")
            for gl in range(NG):
                p.op("vector", lambda e, gl=gl: e.tensor_scalar(out=a[:], in0=cst["iota"][:], scalar1=th[:, gl:gl + 1], scalar2=None, op0=ALU.mult), reads=[b_c, b2, b_tab], writes=[b_a])
                emit_frac_sin(p, a[:], b_a, Sg[:, gl, :], b_tab, None, (ki[:], kf[:], t1[:]))
                p.op("vector", lambda e, gl=gl: e.tensor_scalar(out=Sg[:, gl, :], in0=Sg[:, gl, :], scalar1=cst["sgn2"][:, 0:1], scalar2=None, op0=ALU.mult), reads=[b_tab, b_c], writes=[b_tab])
                p.op("vector", lambda e, gl=gl: e.tensor_scalar(out=a[:], in0=cst["iota"][:], scalar1=th[:, gl:gl + 1], scalar2=0.25, op0=ALU.mult, op1=ALU.add), reads=[b_c, b2, b_tab], writes=[b_a])
                emit_frac_sin(p, a[:], b_a, Ct[:, gl, :], b_tab, None, (ki[:], kf[:], t1[:]))
                p.op("vector", lambda e, gl=gl: e.tensor_scalar(out=Rt[:, gl, :], in0=cst["onesL"][:], scalar1=rho[:, gl:gl + 1], scalar2=None, op0=ALU.mult), reads=[b_c, b_rho, b_tab], writes=[b_tab])
        uf = p.sb([128, S], F32, name="s5uf")
        ub = p.sb([128, S], BF16, name="s5ub")
        b_uf, b_ub = Buf("uf"), Buf("ub")
        p.dma("sync", uf[:], u_d, writes=[b_uf])
        p.op("scalar", lambda e: e.activation(out=ub[:], in_=uf[:], func=AF.Identity), reads=[b_uf], writes=[b_ub])
        carry = p.sb([128, NG], F32, name="s5carry")
        b_carry = [Buf("carry%d" % g) for g in range(NG)]
        for g in range(NG):
            p.op("vector", lambda e, g=g: e.memset(carry[:, g:g + 1], 0.0), writes=[b_carry[g]])
        xA = mk_rot(p, 2, [128, 512], F32, psum=True, name="xA")
        xB = mk_rot(p, 2, [128, 512], F32, psum=True, name="xB")
        sB = mk_rot(p, 2, [128, 512], F32, psum=True, name="sB")
        yps = p.ps([128, 512], F32, name="s5y")
        b_y = Buf("yps")
        t1_r = mk_rot(p, 2, [128, Lc], F32, name="s5t1")
        t2_r = mk_rot(p, 2, [128, Lc], F32, name="s5t2")
        z_r = mk_rot(p, 2, [128, Lc], F32, name="s5z")
        S_r = mk_rot(p, 2, [128, Lc], F32, name="s5S")
        hb_r = mk_rot(p, 2, [128, Lc], BF16, name="s5hb")
        c1_r = mk_rot(p, 2, [128, 1], F32, name="s5c1")
        yv_r = mk_rot(p, 2, [128, Lc], F32, name="s5yv")
        y2_r = mk_rot(p, 2, [128, Lc], F32, name="s5y2")
        for c in range(NTC):
            cs_ = slice(c * Lc, (c + 1) * Lc)
            for gl in range(NG):
                (xa, b_xa), (xb, b_xb), (sb_, b_sb) = xA.next(), xB.next(), sB.next()
                (t1, b_t1), (t2, b_t2), (z, b_z), (St, b_S), (hb, b_hb), (c1, b_c1) = t1_r.next(), t2_r.next(), z_r.next(), S_r.next(), hb_r.next(), c1_r.next()
                p.op("tensor", lambda e, xa=xa, gl=gl, cs_=cs_: e.matmul(xa[:, :Lc], WxA[:, gl, :], ub[:, cs_], start=True, stop=True), reads=[b_W, b_ub], writes=[b_xa])
                p.op("tensor", lambda e, xb=xb, gl=gl, cs_=cs_: e.matmul(xb[:, :Lc], WxB[:, gl, :], ub[:, cs_], start=True, stop=True), reads=[b_W, b_ub], writes=[b_xb])
                p.op("vector", lambda e, xa=xa, t1=t1, gl=gl: e.tensor_tensor(out=t1[:], in0=xa[:, :Lc], in1=Ct[:, gl, :], op=ALU.mult), reads=[b_xa, b_tab], writes=[b_t1])
                p.op("vector", lambda e, xb=xb, t2=t2, gl=gl: e.tensor_tensor(out=t2[:], in0=xb[:, :Lc], in1=Sg[:, gl, :], op=ALU.mult), reads=[b_xb, b_tab], writes=[b_t2])
                p.op("gpsimd", lambda e, t1=t1, t2=t2, z=z: e.tensor_tensor(out=z[:], in0=t1[:], in1=t2[:], op=ALU.add), reads=[b_t1, b_t2], writes=[b_z])
                p.op("vector", lambda e, z=z, St=St, gl=gl: e.tensor_tensor_scan(out=St[:], data0=Rt[:, gl, :], data1=z[:], initial=carry[:, gl:gl + 1], op0=ALU.mult, op1=ALU.add),
                     reads=[b_z, b_tab, b_carry[gl]], writes=[b_S])
                p.op("tensor", lambda e, sb_=sb_, St=St: e.matmul(sb_[:, :Lc], cst["swapP"][:], St[:], start=True, stop=True), reads=[b_S, b_c], writes=[b_sb])
                p.op("gpsimd", lambda e, t1=t1, St=St, gl=gl: e.tensor_tensor(out=t1[:], in0=St[:], in1=Ct[:, gl, :], op=ALU.mult), reads=[b_S, b_tab, b_t1, b_z], writes=[b_t1])
                p.op("vector", lambda e, sb_=sb_, t2=t2, gl=gl: e.tensor_tensor(out=t2[:], in0=sb_[:, :Lc], in1=Sg[:, gl, :], op=ALU.mult), reads=[b_sb, b_tab, b_z], writes=[b_t2])
                p.op("vector", lambda e, t1=t1, t2=t2, hb=hb: e.tensor_tensor(out=hb[:], in0=t1[:], in1=t2[:], op=ALU.subtract), reads=[b_t1, b_t2], writes=[b_hb])
                p.op("vector", lambda e, St=St, c1=c1, gl=gl: e.tensor_tensor(out=c1[:], in0=St[:, Lc - 1:Lc], in1=cL[:, gl:gl + 1], op=ALU.mult), reads=[b_S, b_cL], writes=[b_c1])
                p.op("vector", lambda e, sb_=sb_, c1=c1, gl=gl: e.scalar_tensor_tensor(out=carry[:, gl:gl + 1], in0=sb_[:, Lc - 1:Lc], scalar=nsL[:, gl:gl + 1], in1=c1[:], op0=ALU.mult, op1=ALU.add),
                     reads=[b_sb, b_c1, b_cL], writes=[b_carry[gl]])
                p.op("tensor", lambda e, hb=hb, gl=gl: e.matmul(yps[:, :Lc], Cpad[:, gl, :], hb[:], start=(gl == 0), stop=(gl == NG - 1)), reads=[b_hb, b_C], writes=[b_y])
            (yv, b_yv), (y2, b_y2) = yv_r.next(), y2_r.next()
            p.op("vector", lambda e, yv=yv, cs_=cs_: e.scalar_tensor_tensor(out=yv[:], in0=uf[:, cs_], scalar=prm["d2"][:, 0:1], in1=yps[:, :Lc], op0=ALU.mult, op1=ALU.add),
                 reads=[b_uf, b_p, b_y], writes=[b_yv])
            p.op("scalar", lambda e, yv=yv, y2=y2: e.activation(out=y2[:], in_=yv[:], func=AF.Square), reads=[b_yv], writes=[b_y2])
            p.op("vector", lambda e, y2=y2: e.tensor_scalar(out=y2[:], in0=y2[:], scalar1=0.044715, scalar2=1.0, op0=ALU.mult, op1=ALU.add), reads=[b_y2], writes=[b_y2])
            p.op("vector", lambda e, yv=yv, y2=y2: e.tensor_tensor(out=y2[:], in0=y2[:], in1=yv[:], op=ALU.mult), reads=[b_y2, b_yv], writes=[b_y2])
            p.op("scalar", lambda e, y2=y2: e.activation(out=y2[:], in_=y2[:], func=AF.Tanh, scale=0.7978845608028654), reads=[b_y2], writes=[b_y2])
            p.op("vector", lambda e, yv=yv, y2=y2: e.scalar_tensor_tensor(out=y2[:], in0=y2[:], scalar=1.0, in1=yv[:], op0=ALU.add, op1=ALU.mult), reads=[b_y2, b_yv], writes=[b_y2])
            p.op("vector", lambda e, y2=y2: e.tensor_single_scalar(out=y2[:], in_=y2[:], scalar=0.5, op=ALU.mult), reads=[b_y2], writes=[b_y2])
            p.dma("sync", out_d[:, cs_], y2[:], reads=[b_y2])


def build_s5_test(S):
    nc = bass.Bass("TRN2", target_bir_lowering=False)
    ud = nc.dram_tensor("uT", [128, S], F32, kind="ExternalInput").ap()
    cd, pd = decl_s5(nc, 1)
    od = nc.dram_tensor("yT", [128, S], F32, kind="ExternalOutput").ap()
    with ExitStack() as es:
        p = Prog(nc, es)
        emit_s5_chunk(p, S, ud, cd, pd[0], od)
        p.finish()
        p.emit()
    return nc


def build_mix_even(S):
    nc = bass.Bass("TRN2", target_bir_lowering=False)
    qTd = nc.dram_tensor("qT", [2, 128, S], F32, kind="ExternalInput").ap()
    kTd = nc.dram_tensor("kT", [2, 128, S], F32, kind="ExternalInput").ap()
    vd = nc.dram_tensor("v", [2, S, 128], F32, kind="ExternalInput").ap()
    ud = nc.dram_tensor("uT", [2, 128, S], F32, kind="ExternalInput").ap()
    trid = nc.dram_tensor("tri", [128, 128], F32, kind="ExternalInput").ap()
    onesd = nc.dram_tensor("ones", [128, 128], F32, kind="ExternalInput").ap()
    maskd = nc.dram_tensor("masks", [128, 4 * 512], F32, kind="ExternalInput").ap()
    cd, pd = decl_s5(nc, 2)
    oTd = nc.dram_tensor("oT", [2, 128, S], F32, kind="ExternalOutput").ap()
    yTd = nc.dram_tensor("yT", [2, 128, S], F32, kind="ExternalOutput").ap()
    with ExitStack() as es:
        p = Prog(nc, es)
        with p.scope():
            consts = load_attn_consts(p, trid, onesd, maskd)
            emit_sb_all(p, S, 2, qTd, kTd, vd, consts, oTd)
        for j in range(2):
            emit_s5_chunk(p, S, ud[j], cd, pd[j], yTd[j])
        p.finish()
        p.emit()
    return nc


def build_mix_odd(S):
    nc = bass.Bass("TRN2", target_bir_lowering=False)
    hq = nc.dram_tensor("hq", [2, 128, S], F32, kind="ExternalInput").ap()
    hf = nc.dram_tensor("hf", [2, 128, S], F32, kind="ExternalInput").ap()
    hg = nc.dram_tensor("hg", [2, 128, S], F32, kind="ExternalInput").ap()
    hi = nc.dram_tensor("hi", [2, S, 128], F32, kind="ExternalInput").ap()
    hlb = nc.dram_tensor("hlb", [2, 128, 2], F32, kind="ExternalInput").ap()
    hnw = nc.dram_tensor("hnw", [128, 1], F32, kind="ExternalInput").ap()
    hcd = decl_hgrn_consts(nc)
    qd = nc.dram_tensor("qT", [2, 128, S], F32, kind="ExternalInput").ap()
    kd = nc.dram_tensor("kT", [2, 128, S], F32, kind="ExternalInput").ap()
    qsd = nc.dram_tensor("qsT", [2, 32, S], F32, kind="ExternalInput").ap()
    ksd = nc.dram_tensor("ksT", [2, 32, S], F32, kind="ExternalInput").ap()
    vd = nc.dram_tensor("v", [2, S, 128], F32, kind="ExternalInput").ap()
    mcd = decl_moba_consts(nc, S)
    rope_d = nc.dram_tensor("ropeo", [4, 32, S], F32, kind="ExternalOutput").ap()
    oh = nc.dram_tensor("oh", [2, 128, S], F32, kind="ExternalOutput").ap()
    om = nc.dram_tensor("om", [2, 128, S], F32, kind="ExternalOutput").ap()
    with ExitStack() as es:
        p = Prog(nc, es)
        with p.scope():
            hc = load_hgrn_consts(p, hcd)
            for h in range(2):
                emit_hgrn_head(p, S, hq[h], hf[h], hg[h], hi[h], hlb[h], hnw, hc, oh[h])
        with p.scope():
            cst = load_moba_consts(p, mcd, S)
            srcs = []
            for h in range(2):
                srcs.append((qd[h, 0:32, :], qsd[h]))
                srcs.append((kd[h, 0:32, :], ksd[h]))
            emit_rope_rows(p, S, srcs, mcd["ropec"], mcd["pos"], rope_d)
            for h in range(2):
                emit_moba_head(p, S, qd[h], kd[h], rope_d[2 * h], rope_d[2 * h + 1], vd[h], cst, om[h])
        p.finish()
        p.emit()
    return nc


CFG = {"D": D_MODEL, "S": SEQ, "FF": D_FF, "NH": 16, "n_ffn": 4, "n_in": 4, "n_out": 8, "n_mod": 8, "TT": 512}
MODE = "unfused"
_PROGS = {}


def _prog(key, fn):
    key = (key, tuple(sorted(CFG.items())))
    if key not in _PROGS:
        _PROGS[key] = fn()
    return _PROGS[key]


DEBUG = [False]


_T0 = [None]


def _run(nc, in_maps):
    import time as _time
    if _T0[0] is None:
        _T0[0] = _time.time()
    t_ = _time.time()
    res = run_bass_kernel_spmd(nc, in_maps, core_ids=list(range(len(in_maps))))
    nb = sum(v.nbytes for m in in_maps for v in m.values())
    print("[kernel] launch cores=%d in=%.0fMB took %.1fs (t=%.0fs)" % (len(in_maps), nb / 1e6, _time.time() - t_, _time.time() - _T0[0]), flush=True)
    if DEBUG[0]:
        for k in res.results[0]:
            a = np.concatenate([r[k].reshape(-1) for r in res.results])
            print("DEBUG launch out", k, "finite:", bool(np.isfinite(a).all()), "rms %.4g" % np.sqrt(np.mean(a[np.isfinite(a)] ** 2)), flush=True)
    return res.results


def _vec_layout(rows):
    R, D = rows.shape
    DC = D // 128
    return np.ascontiguousarray(rows.reshape(R, DC, 128).transpose(2, 0, 1).reshape(128, R * DC)).astype(np.float32)


def _c(a):
    return np.ascontiguousarray(a, dtype=np.float32)


def kernel_unfused(x, c, ada_w, ada_table, norm_pre, norm_post, ffn_w1, ffn_w3, ffn_w2,
           ev_w_in, ev_w_out, s5_a_re, s5_a_im, s5_b_re, s5_b_im, s5_c_re, s5_c_im,
           s5_d, s5_log_dt, s5_glu_w, od_w_in, od_w_out, hgrn_lb, hgrn_norm_w):
    f = lambda a: np.asarray(a, dtype=np.float32)
    x, c, ada_w, ada_table = f(x), f(c), f(ada_w), f(ada_table)
    norm_pre, norm_post = f(norm_pre), f(norm_post)
    S, D, FF, NH = CFG["S"], CFG["D"], CFG["FF"], CFG["NH"]
    DC = D // 128
    W = NH * 128
    NMIX = NH // 2
    TT_TOK = CFG["TT"]
    NMOD = CFG["n_mod"]
    NCH = 9 * D // 128 // NMOD
    nc = _prog("mod", lambda: build_mod(DC, NCH))
    cT = _c(c.reshape(DC, 128).T)
    res = _run(nc, [{"cT": cT, "adaw": _c(ada_w[:, i * NCH * 128:(i + 1) * NCH * 128])} for i in range(NMOD)])
    mod_shared = np.concatenate([r["modo"].T.reshape(-1) for r in res]).reshape(9, D)

    xT = _c(x.reshape(S, D).T)

    def vecs_gains(layer, s):
        rows = np.concatenate([mod_shared[3 * s:3 * s + 3], ada_table[layer, 3 * s:3 * s + 3]], axis=0)
        return _vec_layout(rows), _vec_layout(np.stack([norm_pre[layer, s], norm_post[layer, s]]))

    def split_tok(a, n):
        w = a.shape[1] // n
        return [_c(a[:, i * w:(i + 1) * w]) for i in range(n)]

    def run_ffn(xT, layer, j, s):
        n = CFG["n_ffn"]
        nc = _prog("ffn", lambda: build_sandwich("ffn", DC, FF // 128, S // n, TT_TOK, 0.5))
        vecs, gains = vecs_gains(layer, s)
        w1, w3, w2 = f(ffn_w1[layer, j]), f(ffn_w3[layer, j]), f(ffn_w2[layer, j])
        xs = split_tok(xT, n)
        res = _run(nc, [{"xT": xs[i], "w1": w1, "w3": w3, "w2": w2, "vecs": vecs, "gains": gains} for i in range(n)])
        return np.concatenate([r["yT"] for r in res], axis=1)

    def run_in(xT, layer, win, key):
        n = CFG["n_in"]
        NOUT = win.shape[1]
        nc = _prog(key, lambda: build_sandwich("in", DC, 0, S // n, TT_TOK, 1.0, NOUT=NOUT))
        vecs, gains = vecs_gains(layer, 1)
        xs = split_tok(xT, n)
        res = _run(nc, [{"xT": xs[i], "win": win, "vecs": vecs, "gains": gains} for i in range(n)])
        return np.concatenate([r["pT"] for r in res], axis=1)

    def run_out(xT, oT, layer, wout, gluw, key):
        n = CFG["n_out"]
        nc = _prog(key, lambda: build_sandwich("out", DC, DC, S // n, TT_TOK, 1.0, glu=gluw is not None))
        vecs, gains = vecs_gains(layer, 1)
        xs, os_ = split_tok(xT, n), split_tok(oT, n)
        ins = []
        for i in range(n):
            d = {"xT": xs[i], "oT": os_[i], "w2": wout, "vecs": vecs, "gains": gains}
            if gluw is not None:
                d["gluw"] = gluw
            ins.append(d)
        res = _run(nc, ins)
        return np.concatenate([r["yT"] for r in res], axis=1)

    for layer in range(2):
        xT = run_ffn(xT, layer, 0, 0)
        if layer % 2 == 0:
            e = layer // 2
            pT = run_in(xT, layer, f(ev_w_in[e]), "in_even")
            nc = _prog("mix_even", lambda: build_mix_even(S))
            tri, ones, masks = host_attn_consts(512, strict=True)
            s5c = host_s5_consts()
            ins = []
            for i in range(NMIX):
                hs = [2 * i, 2 * i + 1]
                d = {"qT": _c(np.stack([pT[128 * h:128 * h + 128] for h in hs])),
                     "kT": _c(np.stack([pT[W + 128 * h:W + 128 * h + 128] for h in hs])),
                     "v": _c(np.stack([pT[2 * W + 128 * h:2 * W + 128 * h + 128].T for h in hs])),
                     "uT": _c(np.stack([pT[3 * W + 128 * h:3 * W + 128 * h + 128] for h in hs])),
                     "tri": tri, "ones": ones, "masks": masks}
                for k_, v_ in s5c.items():
                    d["s_" + k_] = v_
                for j, h in enumerate(hs):
                    prm = host_s5_params(f(s5_a_re[e]), f(s5_a_im[e]), f(s5_b_re[e]), f(s5_b_im[e]), f(s5_c_re[e]), f(s5_c_im[e]),
                                         f(s5_d[e]), f(s5_log_dt[e]), 8 * h)
                    for k_, v_ in prm.items():
                        d["s%d_%s" % (j, k_)] = v_
                ins.append(d)
            res = _run(nc, ins)
            oT = np.concatenate([np.concatenate([r["oT"][0], r["oT"][1]], axis=0) for r in res]
                                + [np.concatenate([r["yT"][0], r["yT"][1]], axis=0) for r in res], axis=0)
            xT = run_out(xT, oT, layer, f(ev_w_out[e]), f(s5_glu_w[e]), "out_even")
        else:
            o = layer // 2
            w = f(od_w_in[o])
            swap_cols = []
            for base in (4 * W, 5 * W):
                for h in range(NH):
                    b0 = base + 128 * h
                    swap_cols += list(range(b0 + 16, b0 + 32)) + list(range(b0, b0 + 16))
            w_ext = np.concatenate([w, w[:, swap_cols]], axis=1)
            pT = run_in(xT, layer, _c(w_ext), "in_odd")
            nc = _prog("mix_odd", lambda: build_mix_odd(S))
            hc = host_hgrn_consts()
            mc = host_moba_consts(S)
            lb = f(hgrn_lb)
            ins = []
            for i in range(NMIX):
                hs = [2 * i, 2 * i + 1]
                blk = lambda base, h: pT[base + 128 * h: base + 128 * h + 128]
                d = {"hq": _c(np.stack([blk(0, h) for h in hs])), "hf": _c(np.stack([blk(W, h) for h in hs])),
                     "hi": _c(np.stack([blk(2 * W, h).T for h in hs])), "hg": _c(np.stack([blk(3 * W, h) for h in hs])),
                     "hlb": _c(np.stack([lb[:, 128 * h:128 * h + 128].T for h in hs])),
                     "hnw": _c(f(hgrn_norm_w[o])[:, None]),
                     "qT": _c(np.stack([blk(4 * W, h) for h in hs])), "kT": _c(np.stack([blk(5 * W, h) for h in hs])),
                     "v": _c(np.stack([blk(6 * W, h).T for h in hs])),
                     "qsT": _c(np.stack([pT[7 * W + 32 * h:7 * W + 32 * h + 32] for h in hs])),
                     "ksT": _c(np.stack([pT[7 * W + 32 * NH + 32 * h:7 * W + 32 * NH + 32 * h + 32] for h in hs]))}
                for k_, v_ in hc.items():
                    d["h_" + k_] = v_
                for k_, v_ in mc.items():
                    d["c_" + k_] = v_
                ins.append(d)
            if DEBUG[0]:
                lbv = 1 / (1 + np.exp(-(lb[1] - lb[0])))
                ff = pT[W:2 * W].T
                fv = lbv + (1 - lbv) / (1 + np.exp(-ff))
                lg = np.log(fv).reshape(S // 64, 64, W).sum(1)
                print("DEBUG hgrn: min lb %.4f, min 64-step log decay %.2f" % (lbv.min(), lg.min()), flush=True)
            res = _run(nc, ins)
            oT = np.concatenate([np.concatenate([r["oh"][0], r["oh"][1]], axis=0) for r in res]
                                + [np.concatenate([r["om"][0], r["om"][1]], axis=0) for r in res], axis=0)
            xT = run_out(xT, oT, layer, f(od_w_out[o]), None, "out_odd")
        xT = run_ffn(xT, layer, 1, 2)
    return np.ascontiguousarray(xT.T).reshape(1, S, D).astype(np.float32)


def build_fused(D, S, FF, NH, TT=512):
    nc = bass.Bass("TRN2", target_bir_lowering=False)
    DC, FC, W = D // 128, FF // 128, NH * 128
    NCK = W // 128
    NOUT_E, NOUT_O = 4 * W, 7 * W + 64 * NH
    ein = lambda name, shape: nc.dram_tensor(name, shape, F32, kind="ExternalInput").ap()
    xT = ein("xT", [D, S])
    cT = ein("cT", [128, DC])
    adaw = ein("adaw", [D, 9 * D])
    tabs = ein("tabs", [128, 18 * DC])
    npre = ein("npre", [128, 6 * DC])
    npost = ein("npost", [128, 6 * DC])
    fw = {}
    for l in range(2):
        for j in range(2):
            fw[(l, j)] = (ein("w1_%d%d" % (l, j), [D, FF]), ein("w3_%d%d" % (l, j), [D, FF]), ein("w2_%d%d" % (l, j), [FF, D]))
    win_e, win_o = ein("win_e", [D, NOUT_E]), ein("win_o", [D, NOUT_O])
    wout_e, wout_o, gluw = ein("wout_e", [2 * W, D]), ein("wout_o", [2 * W, D]), ein("gluw", [W, W])
    trid, onesd, maskd = ein("tri", [128, 128]), ein("ones", [128, 128]), ein("masks", [128, 2048])
    s5cd, s5pd = decl_s5(nc, NCK)
    hcd = decl_hgrn_consts(nc)
    mcd = decl_moba_consts(nc, S)
    hlb = ein("hlb", [NH, 128, 2])
    hnw = ein("hnw", [128, 1])
    yT = nc.dram_tensor("yT", [D, S], F32, kind="ExternalOutput").ap()
    xA = nc.dram_tensor("xA", [D, S], F32).ap()
    xB = nc.dram_tensor("xB", [D, S], F32).ap()
    NSEG = (max(NOUT_E, NOUT_O) + W - 1) // W
    pseg = [nc.dram_tensor("pTs%d" % i, [W, S], F32).ap() for i in range(NSEG)]

    class _PT:
        def __getitem__(self, key):
            rs, cs = key
            r0, r1 = rs.start, rs.stop
            sg = r0 // W
            assert (r1 - 1) // W == sg, (r0, r1)
            return pseg[sg][r0 - sg * W:r1 - sg * W, cs]
    pT = _PT()
    oT = nc.dram_tensor("oTs", [2 * W, S], F32).ap()
    modo = nc.dram_tensor("modos", [128, 9 * DC], F32).ap()
    rope_d = nc.dram_tensor("ropes", [2 * NH, 32, S], F32).ap()
    v3 = lambda a: a.rearrange("(c p) t -> p c t", p=128)

    def vg(layer, s):
        return {"vec_pieces": [(0, 3 * DC, modo[:, 3 * s * DC:3 * s * DC + 3 * DC]), (3 * DC, 3 * DC, tabs[:, (layer * 9 + 3 * s) * DC:(layer * 9 + 3 * s + 3) * DC])],
                "gain_pieces": [(0, DC, npre[:, (layer * 3 + s) * DC:(layer * 3 + s + 1) * DC]), (DC, DC, npost[:, (layer * 3 + s) * DC:(layer * 3 + s + 1) * DC])]}

    with ExitStack() as es:
        p = Prog(nc, es)

        def ffn(src, dst, layer, j, s):
            with p.scope():
                T = vg(layer, s)
                w1, w3, w2 = fw[(layer, j)]
                T.update({"xT_v": v3(src), "yT_v": v3(dst), "w1_v": v3(w1), "w3_v": v3(w3), "w2_v": v3(w2)})
                _sandwich_body(p, "ffn", DC, FC, S, TT, 0.5, T)

        def proj_in(src, layer, win, NOUT):
            with p.scope():
                T = vg(layer, 1)
                T.update({"xT_v": v3(src), "win_v": v3(win), "pT_chunk": (lambda m: pT[128 * m:128 * m + 128, :])})
                _sandwich_body(p, "in", DC, 0, S, TT, 1.0, T, NOUT=NOUT)

        def proj_out(src, dst, layer, wout, glu):
            with p.scope():
                T = vg(layer, 1)
                T.update({"xT_v": v3(src), "yT_v": v3(dst), "oT_v": v3(oT), "w2_v": v3(wout)})
                if glu:
                    T["gluw_v"] = v3(gluw)
                _sandwich_body(p, "out", DC, 2 * W // 128, S, TT, 1.0, T, glu=glu)

        with p.scope():
            _mod_body(p, DC, 9 * DC, cT, v3(adaw), modo)
        ffn(xT, xA, 0, 0, 0)
        proj_in(xA, 0, win_e, NOUT_E)
        with p.scope():
            consts = load_attn_consts(p, trid, onesd, maskd)
            identb = p.sb([128, 128], BF16, name="identb_f")
            b_id = Buf("identb")
            p.dma("gpsimd", identb[:], hcd["ident"], writes=[b_id])
            heads = [dict(q=pT[128 * h:128 * h + 128, :], k=pT[W + 128 * h:W + 128 * h + 128, :], v_fm=pT[2 * W + 128 * h:2 * W + 128 * h + 128, :],
                          out=oT[128 * h:128 * h + 128, :]) for h in range(NH)]
            emit_sb_heads(p, S, heads, consts, identb[:], b_id)
        for ck in range(NCK):
            emit_s5_chunk(p, S, pT[3 * W + 128 * ck:3 * W + 128 * ck + 128, :], s5cd, s5pd[ck], oT[W + 128 * ck:W + 128 * ck + 128, :])
        proj_out(xA, xB, 0, wout_e, True)
        ffn(xB, xA, 0, 1, 2)
        ffn(xA, xB, 1, 0, 0)
        proj_in(xB, 1, win_o, NOUT_O)
        with p.scope():
            hc = load_hgrn_consts(p, hcd)
            for h in range(NH):
                r = lambda base: pT[base + 128 * h:base + 128 * h + 128, :]
                emit_hgrn_head(p, S, r(0), r(W), r(3 * W), None, hlb[h], hnw, hc, oT[128 * h:128 * h + 128, :], i_fm=r(2 * W))
        with p.scope():
            cst = load_moba_consts(p, mcd, S)
            identb = p.sb([128, 128], BF16, name="identb_m")
            b_id = Buf("identb")
            p.dma("gpsimd", identb[:], hcd["ident"], writes=[b_id])
            srcs = []
            for h in range(NH):
                srcs.append((pT[4 * W + 128 * h:4 * W + 128 * h + 32, :], pT[7 * W + 32 * h:7 * W + 32 * h + 32, :]))
                srcs.append((pT[5 * W + 128 * h:5 * W + 128 * h + 32, :], pT[7 * W + 32 * NH + 32 * h:7 * W + 32 * NH + 32 * h + 32, :]))
            emit_rope_rows(p, S, srcs, mcd["ropec"], mcd["pos"], rope_d)
            for h in range(NH):
                emit_moba_head(p, S, pT[4 * W + 128 * h:4 * W + 128 * h + 128, :], pT[5 * W + 128 * h:5 * W + 128 * h + 128, :],
                               rope_d[2 * h], rope_d[2 * h + 1], None, cst, oT[W + 128 * h:W + 128 * h + 128, :],
                               v_fm=pT[6 * W + 128 * h:6 * W + 128 * h + 128, :], identb=identb[:], b_id=b_id)
        proj_out(xB, xA, 1, wout_o, False)
        ffn(xA, yT, 1, 1, 2)
        p.finish()
        p.emit()
    return nc


def kernel_fused(x, c, ada_w, ada_table, norm_pre, norm_post, ffn_w1, ffn_w3, ffn_w2,
                 ev_w_in, ev_w_out, s5_a_re, s5_a_im, s5_b_re, s5_b_im, s5_c_re, s5_c_im,
                 s5_d, s5_log_dt, s5_glu_w, od_w_in, od_w_out, hgrn_lb, hgrn_norm_w):
    f = lambda a: np.asarray(a, dtype=np.float32)
    S, D, FF, NH = CFG["S"], CFG["D"], CFG["FF"], CFG["NH"]
    DC, W = D // 128, NH * 128
    nc = _prog("fused", lambda: build_fused(D, S, FF, NH, CFG["TT"]))
    ins = {"xT": _c(f(x).reshape(S, D).T), "cT": _c(f(c).reshape(DC, 128).T), "adaw": _c(f(ada_w)),
           "tabs": _vec_layout(f(ada_table).reshape(18, D)), "npre": _vec_layout(f(norm_pre).reshape(6, D)), "npost": _vec_layout(f(norm_post).reshape(6, D))}
    for l in range(2):
        for j in range(2):
            ins["w1_%d%d" % (l, j)] = _c(ffn_w1[l, j])
            ins["w3_%d%d" % (l, j)] = _c(ffn_w3[l, j])
            ins["w2_%d%d" % (l, j)] = _c(ffn_w2[l, j])
    w = f(od_w_in[0])
    swap_cols = []
    for base in (4 * W, 5 * W):
        for h in range(NH):
            b0 = base + 128 * h
            swap_cols += list(range(b0 + 16, b0 + 32)) + list(range(b0, b0 + 16))
    ins["win_e"] = _c(ev_w_in[0])
    ins["win_o"] = _c(np.concatenate([w, w[:, swap_cols]], axis=1))
    ins["wout_e"], ins["wout_o"], ins["gluw"] = _c(ev_w_out[0]), _c(od_w_out[0]), _c(s5_glu_w[0])
    tri, ones, masks = host_attn_consts(512, strict=True)
    ins.update({"tri": tri, "ones": ones, "masks": masks})
    for k_, v_ in host_s5_consts().items():
        ins["s_" + k_] = v_
    for ck in range(W // 128):
        prm = host_s5_params(f(s5_a_re[0]), f(s5_a_im[0]), f(s5_b_re[0]), f(s5_b_im[0]), f(s5_c_re[0]), f(s5_c_im[0]), f(s5_d[0]), f(s5_log_dt[0]), 8 * ck)
        for k_, v_ in prm.items():
            ins["s%d_%s" % (ck, k_)] = v_
    for k_, v_ in host_hgrn_consts().items():
        ins["h_" + k_] = v_
    for k_, v_ in host_moba_consts(S).items():
        ins["c_" + k_] = v_
    lb = f(hgrn_lb)
    ins["hlb"] = _c(np.stack([lb[:, 128 * h:128 * h + 128].T for h in range(NH)]))
    ins["hnw"] = _c(f(hgrn_norm_w[0])[:, None])
    res = _run(nc, [ins])
    return np.ascontiguousarray(res[0]["yT"].T).reshape(1, S, D).astype(np.float32)


def kernel(**inputs):
    if MODE == "fused":
        return kernel_fused(**inputs)
    return kernel_unfused(**inputs)
```
